# Optimizing a Trainium2 kernel written in Bass

```python
import math
import jax, jax.numpy as jnp
from jax import lax
import numpy as np

D_MODEL = 1024
BATCH = 2
SEQ = 16384
DEPTH = 4

CHUNK = 128
A_GROUPS = 4
A_CH = 128
A_WIDTH = A_GROUPS * A_CH
B_GROUPS = 4
B_WIDTH = 512
CONV_W = 3
MIX_IN = 2 * A_WIDTH + 3 * B_WIDTH
MIX_OUT = A_WIDTH + B_WIDTH
N_HEADS = 16
N_KV = 2
HEAD_DIM = 64
GQA_GROUP = N_HEADS // N_KV
WINDOW = 128
QKV_DIM = (N_HEADS + 2 * N_KV) * HEAD_DIM
ATT_OUT = N_HEADS * HEAD_DIM
N_BUCKETS = 32
MAX_DISTANCE = 128
N_GROUPS = 4
EXPERTS_PER_GROUP = 8
N_EXPERTS = N_GROUPS * EXPERTS_PER_GROUP
TOP_K = 2
D_EXPERT = 512
ROW_BLOCK = 128
ALPHA = (2 * DEPTH) ** 0.25
BETA = (8 * DEPTH) ** -0.25
LN_EPS = 1e-5
N_EVEN = (DEPTH + 1) // 2
N_ODD = DEPTH // 2

kernel_name = 'hybrid_gmlp_conv_swa_hmoe_deepnorm'


def layer_norm(x, g, b):
    xf = x.astype(jnp.float32)
    mu = jnp.mean(xf, axis=-1, keepdims=True)
    var = jnp.mean(jnp.square(xf - mu), axis=-1, keepdims=True)
    y = (xf - mu) * lax.rsqrt(var + LN_EPS)
    return (y * g.astype(jnp.float32) + b.astype(jnp.float32)).astype(x.dtype)


def t5_bucket(rel):
    n = jnp.maximum(rel, 0)
    max_exact = N_BUCKETS // 2
    nf = jnp.maximum(n, 1).astype(jnp.float32)
    large = max_exact + (jnp.log(nf / max_exact) / math.log(MAX_DISTANCE / max_exact)
                         * (N_BUCKETS - max_exact)).astype(jnp.int32)
    large = jnp.minimum(large, N_BUCKETS - 1)
    return jnp.where(n < max_exact, n, large)


def gating_conv_mixer(x, w_in, ln_g, ln_b, w_sp, b_sp, conv_w, w_out):
    bsz, s, _ = x.shape
    h = x @ w_in
    u = jax.nn.gelu(h[..., :A_WIDTH], approximate=False)
    v = jax.nn.gelu(h[..., A_WIDTH:2 * A_WIDTH], approximate=False)
    o = 2 * A_WIDTH
    g_b = h[..., o:o + B_WIDTH]
    g_c = h[..., o + B_WIDTH:o + 2 * B_WIDTH]
    hb = h[..., o + 2 * B_WIDTH:o + 3 * B_WIDTH]
    v = layer_norm(v, ln_g, ln_b)
    v = v.reshape(bsz, s // CHUNK, CHUNK, A_GROUPS, A_CH)
    causal = jnp.tril(jnp.ones((CHUNK, CHUNK), dtype=bool))
    ws = jnp.where(causal[None], w_sp, 0)
    sv = jnp.einsum('gij,bnjgc->bnigc', ws, v) + b_sp.T[:, :, None]
    y_a = u * sv.reshape(bsz, s, A_WIDTH)
    z = g_c * hb
    zp = jnp.pad(z, ((0, 0), (CONV_W - 1, 0), (0, 0)))
    conv = conv_w[0] * zp[:, 0:s]
    for k in range(1, CONV_W):
        conv = conv + conv_w[k] * zp[:, k:k + s]
    y_b = g_b * conv
    return jnp.concatenate([y_a, y_b], axis=-1) @ w_out


def sliding_window_attention(x, w_qkv, b_qkv, sinks, w_o, b_o, rel_table):
    bsz, s, _ = x.shape
    nb = s // CHUNK
    qkv = x @ w_qkv + b_qkv
    q = qkv[..., :ATT_OUT] * (HEAD_DIM ** -0.5)
    k = qkv[..., ATT_OUT:ATT_OUT + N_KV * HEAD_DIM].reshape(bsz, s, N_KV, HEAD_DIM)
    v = qkv[..., ATT_OUT + N_KV * HEAD_DIM:].reshape(bsz, s, N_KV, HEAD_DIM)
    q = q.reshape(bsz, nb, CHUNK, N_KV, GQA_GROUP, HEAD_DIM)

    def band(t):
        tp = jnp.pad(t, ((0, 0), (CHUNK, 0), (0, 0), (0, 0)))
        tp = tp.reshape(bsz, nb + 1, CHUNK, N_KV, HEAD_DIM)
        return jnp.concatenate([tp[:, :-1], tp[:, 1:]], axis=2)

    kw, vw = band(k), band(v)
    a = jnp.arange(CHUNK)[:, None]
    c = jnp.arange(2 * CHUNK)[None, :]
    rel = a + CHUNK - c
    bias = rel_table[t5_bucket(rel)].astype(jnp.float32)
    bias = jnp.transpose(bias, (2, 0, 1)).reshape(N_KV, GQA_GROUP, CHUNK, 2 * CHUNK)
    win = (rel >= 0) & (rel < WINDOW)
    key_pos = jnp.arange(nb)[:, None, None] * CHUNK - CHUNK + c[None]
    mask = win[None] & (key_pos >= 0)

    sc = jnp.einsum('bnqkgd,bnckd->bnkgqc', q, kw).astype(jnp.float32) + bias
    sc = jnp.where(mask[None, :, None, None], sc, jnp.finfo(jnp.float32).min)
    sink = sinks.astype(jnp.float32).reshape(1, 1, N_KV, GQA_GROUP, 1)
    m = jnp.maximum(jnp.max(sc, axis=-1), sink)
    p = jnp.exp(sc - m[..., None])
    denom = jnp.sum(p, axis=-1) + jnp.exp(sink - m)
    probs = (p / denom[..., None]).astype(vw.dtype)
    o = jnp.einsum('bnkgqc,bnckd->bnqkgd', probs, vw).reshape(bsz, s, ATT_OUT)
    return o @ w_o + b_o


def hierarchical_moe(x, w_grp, w_exp, wg, wu, wd):
    bsz, s, d = x.shape
    t = bsz * s
    xf = x.reshape(t, d)
    g_logits = (xf @ w_grp).astype(jnp.float32)
    g_prob = jax.nn.softmax(g_logits, axis=-1)
    g_idx = jnp.argmax(g_logits, axis=-1)
    g_p = jnp.take_along_axis(g_prob, g_idx[:, None], axis=1)[:, 0]
    e_logits = (xf @ w_exp).astype(jnp.float32).reshape(t, N_GROUPS, EXPERTS_PER_GROUP)
    e_sel = jnp.take_along_axis(e_logits, g_idx[:, None, None], axis=1)[:, 0]
    e_prob = jax.nn.softmax(e_sel, axis=-1)
    top_p, top_i = lax.top_k(e_prob, TOP_K)
    top_p = top_p / jnp.sum(top_p, axis=-1, keepdims=True)
    gates = (g_p[:, None] * top_p).reshape(-1)
    eid = (g_idx[:, None] * EXPERTS_PER_GROUP + top_i).reshape(-1).astype(jnp.int32)
    tok = jnp.repeat(jnp.arange(t, dtype=jnp.int32), TOP_K)
    n = t * TOP_K
    counts = jnp.zeros((N_EXPERTS,), jnp.int32).at[eid].add(1)
    padded = (counts + ROW_BLOCK - 1) // ROW_BLOCK * ROW_BLOCK
    pad_end = jnp.cumsum(padded)
    pad_start = pad_end - padded
    start = jnp.cumsum(counts) - counts
    order = jnp.argsort(eid, stable=True)
    se = eid[order]
    dest = pad_start[se] + jnp.arange(n, dtype=jnp.int32) - start[se]
    p_rows = n + N_EXPERTS * ROW_BLOCK
    nblk = p_rows // ROW_BLOCK
    row_tok = jnp.full((p_rows,), t, jnp.int32).at[dest].set(tok[order])
    row_gate = jnp.zeros((p_rows,), jnp.float32).at[dest].set(gates[order])
    blk_e = jnp.minimum(jnp.searchsorted(pad_end, jnp.arange(nblk, dtype=jnp.int32) * ROW_BLOCK,
                                         side='right'), N_EXPERTS - 1)
    xs = jnp.concatenate([xf, jnp.zeros((1, d), xf.dtype)], axis=0)[row_tok]
    xs = xs.reshape(nblk, ROW_BLOCK, d)

    def run_block(args):
        xb, e = args
        hid = jax.nn.silu(xb @ wg[e]) * (xb @ wu[e])
        return hid @ wd[e]

    ys = lax.map(run_block, (xs, blk_e)).reshape(p_rows, d)
    out = jax.ops.segment_sum(ys * row_gate[:, None].astype(ys.dtype), row_tok,
                              num_segments=t + 1)[:t]
    return out.reshape(bsz, s, d)


def setup_inputs(seed: int = 0) -> dict:
    key = jax.random.key(seed)
    ks = jax.random.split(key, 24)
    f32 = jnp.float32

    def nrm(k, shape, scale):
        return jax.random.normal(k, shape, f32) * scale

    return {
        'x': nrm(ks[0], (BATCH, SEQ, D_MODEL), 1.0),
        'rel_bias_table': nrm(ks[1], (N_BUCKETS, N_HEADS), 0.2),
        'mix_w_in': nrm(ks[2], (N_EVEN, D_MODEL, MIX_IN), D_MODEL ** -0.5),
        'gmlp_ln_g': 1.0 + nrm(ks[3], (N_EVEN, A_WIDTH), 0.02),
        'gmlp_ln_b': nrm(ks[4], (N_EVEN, A_WIDTH), 0.02),
        'gmlp_w_spatial': nrm(ks[5], (N_EVEN, A_GROUPS, CHUNK, CHUNK), 0.5 * CHUNK ** -0.5),
        'gmlp_b_spatial': 1.0 + nrm(ks[6], (N_EVEN, A_GROUPS, CHUNK), 0.02),
        'conv_w': nrm(ks[7], (N_EVEN, CONV_W, B_WIDTH), CONV_W ** -0.5),
        'mix_w_out': nrm(ks[8], (N_EVEN, MIX_OUT, D_MODEL), BETA * MIX_OUT ** -0.5),
        'attn_w_qkv': nrm(ks[9], (N_ODD, D_MODEL, QKV_DIM), D_MODEL ** -0.5),
        'attn_b_qkv': nrm(ks[10], (N_ODD, QKV_DIM), 0.02),
        'attn_sinks': nrm(ks[11], (N_ODD, N_HEADS), 0.5),
        'attn_w_o': nrm(ks[12], (N_ODD, ATT_OUT, D_MODEL), BETA * ATT_OUT ** -0.5),
        'attn_b_o': nrm(ks[13], (N_ODD, D_MODEL), 0.02),
        'ln1_g': 1.0 + nrm(ks[14], (DEPTH, D_MODEL), 0.02),
        'ln1_b': nrm(ks[15], (DEPTH, D_MODEL), 0.02),
        'ln2_g': 1.0 + nrm(ks[16], (DEPTH, D_MODEL), 0.02),
        'ln2_b': nrm(ks[17], (DEPTH, D_MODEL), 0.02),
        'router_group': nrm(ks[18], (DEPTH, D_MODEL, N_GROUPS), D_MODEL ** -0.5),
        'router_expert': nrm(ks[19], (DEPTH, D_MODEL, N_EXPERTS), D_MODEL ** -0.5),
        'expert_w_gate': nrm(ks[20], (DEPTH, N_EXPERTS, D_MODEL, D_EXPERT), D_MODEL ** -0.5),
        'expert_w_up': nrm(ks[21], (DEPTH, N_EXPERTS, D_MODEL, D_EXPERT), D_MODEL ** -0.5),
        'expert_w_down': nrm(ks[22], (DEPTH, N_EXPERTS, D_EXPERT, D_MODEL), BETA * D_EXPERT ** -0.5),
    }


def reference(x, rel_bias_table, mix_w_in, gmlp_ln_g, gmlp_ln_b, gmlp_w_spatial,
              gmlp_b_spatial, conv_w, mix_w_out, attn_w_qkv, attn_b_qkv, attn_sinks,
              attn_w_o, attn_b_o, ln1_g, ln1_b, ln2_g, ln2_b, router_group,
              router_expert, expert_w_gate, expert_w_up, expert_w_down):
    for l in range(DEPTH):
        i = l // 2
        if l % 2 == 0:
            m = gating_conv_mixer(x, mix_w_in[i], gmlp_ln_g[i], gmlp_ln_b[i],
                                  gmlp_w_spatial[i], gmlp_b_spatial[i], conv_w[i],
                                  mix_w_out[i])
        else:
            m = sliding_window_attention(x, attn_w_qkv[i], attn_b_qkv[i], attn_sinks[i],
                                         attn_w_o[i], attn_b_o[i], rel_bias_table)
        x = layer_norm(ALPHA * x + m, ln1_g[l], ln1_b[l])
        f = hierarchical_moe(x, router_group[l], router_expert[l], expert_w_gate[l],
                             expert_w_up[l], expert_w_down[l])
        x = layer_norm(ALPHA * x + f, ln2_g[l], ln2_b[l])
    return x
```

```python
import numpy as np
from contextlib import ExitStack
import concourse.bass as bass
import concourse.mybir as mybir
from concourse.bass_utils import run_bass_kernel_spmd

F32 = mybir.dt.float32
BF16 = mybir.dt.bfloat16
I32 = mybir.dt.int32
AF = mybir.ActivationFunctionType
ALU = mybir.AluOpType
AX = mybir.AxisListType

NCORES = 8
D = 1024
SEQ = 16384
DEPTH = 4
HALO = 3
OWN = 32
NCH = HALO + OWN
TOK = NCH * 128
CAP = 384
RB = CAP // 128
NEXP = 32
XS_ROWS = NEXP * CAP
ALPHA = float((2 * DEPTH) ** 0.25)
EPS = 1e-5
NEG = -30000.0
SAME_ENGINE_SYNC = True


class Res:
    __slots__ = ("name", "w", "r", "psum", "small")

    def __init__(self, name, psum=False, small=False):
        self.name = name
        self.psum = psum
        self.small = small
        self.w = None
        self.r = {}


class DSem:
    def __init__(self, sem):
        self.sem = sem
        self.total = 0


class Eng:
    def __init__(self, name, e, sem):
        self.name = name
        self.e = e
        self.sem = sem
        self.cnt = 0
        self.seen = {}

    def wait(self, tok, small=True):
        sem, val = tok
        if sem is self.sem and (self.name == "pe" or not SAME_ENGINE_SYNC or not small):
            return
        k = id(sem)
        if self.seen.get(k, 0) >= val:
            return
        self.e.wait_ge(sem, val)
        self.seen[k] = val


def _deps(reads, writes):
    toks = []
    for r in reads:
        if r.w is not None:
            toks.append((r.w, r.small))
    for w in writes:
        if w.w is not None:
            toks.append((w.w, w.small))
        toks.extend((t, w.small) for t in w.r.values())
    return toks


def _commit(tok, reads, writes):
    for r in reads:
        r.r[id(tok[0])] = tok
    for w in writes:
        w.w = tok
        w.r = {}


def op(E, reads, writes, fn):
    writes = list(writes) + [r for r in reads if r.psum and r not in writes]
    for t, small in _deps(reads, writes):
        E.wait(t, small)
    inst = fn()
    E.cnt += 1
    inst.then_inc(E.sem, 1)
    _commit((E.sem, E.cnt), reads, writes)


def dma(Q, ds, reads, writes, fn, extra=()):
    for t, _sm in list(_deps(reads, writes)) + [(x, True) for x in extra]:
        Q.wait(t)
    inst = fn()
    ds.total += 16
    inst.then_inc(ds.sem, 16)
    tok = (ds.sem, ds.total)
    _commit(tok, reads, writes)
    return tok


def build(nlayers=DEPTH, dbg=False):
    nc = bass.Bass("TRN2", target_bir_lowering=False)
    es = ExitStack()

    def dram_in(name, shape, dt=F32):
        return nc.dram_tensor(name, list(shape), dt, kind="ExternalInput").ap()

    x_in = dram_in("x_in", [TOK, D])
    mix_w_in = dram_in("mix_w_in", [2, D, 2560])
    mix_w_out = dram_in("mix_w_out", [2, D, D])
    gmlp_ln_g = dram_in("gmlp_ln_g", [2, 512])
    gmlp_ln_b = dram_in("gmlp_ln_b", [2, 512])
    gmlp_w_sp = dram_in("gmlp_w_spatial", [2, 4, 128, 128])
    gmlp_b_sp = dram_in("gmlp_b_spatial", [2, 4, 128])
    conv_w = dram_in("conv_w", [2, 3, 512])
    attn_w_qkv = dram_in("attn_w_qkv", [2, D, 1280])
    attn_b_qkv = dram_in("attn_b_qkv", [2, 1280])
    attn_sinks = dram_in("attn_sinks", [2, 16])
    attn_w_o = dram_in("attn_w_o", [2, D, D])
    attn_b_o = dram_in("attn_b_o", [2, D])
    ln1_g = dram_in("ln1_g", [DEPTH, D])
    ln1_b = dram_in("ln1_b", [DEPTH, D])
    ln2_g = dram_in("ln2_g", [DEPTH, D])
    ln2_b = dram_in("ln2_b", [DEPTH, D])
    router_group = dram_in("router_group", [DEPTH, D, 4])
    router_expert = dram_in("router_expert", [DEPTH, D, 32])
    w_gate = dram_in("expert_w_gate", [DEPTH, NEXP, D, 512])
    w_up = dram_in("expert_w_up", [DEPTH, NEXP, D, 512])
    w_down = dram_in("expert_w_down", [DEPTH, NEXP, 512, D])
    attn_bias = dram_in("attn_bias", [128, 16, 256])
    c_ident = dram_in("c_ident", [128, 128])
    c_ustrict = dram_in("c_ustrict", [128, 128])
    c_causal = dram_in("c_causal", [128, 128])
    c_ecoff = dram_in("c_ecoff", [128, 32])
    c_flags = dram_in("c_flags", [128, 4])

    out = nc.dram_tensor("out", [OWN * 128, D], F32, kind="ExternalOutput").ap()
    dbg_out = nc.dram_tensor("dbg", [DEPTH, 1024, D], F32, kind="ExternalOutput").ap() if dbg else None
    XS = nc.dram_tensor("xs_scr", [XS_ROWS + 128, D], BF16, kind="Internal").ap()
    YS = nc.dram_tensor("ys_scr", [XS_ROWS + 128, D], F32, kind="Internal").ap()
    X1 = nc.dram_tensor("x1_scr", [TOK, D], F32, kind="Internal").ap()
    X2 = nc.dram_tensor("x2_scr", [TOK, D], F32, kind="Internal").ap()

    def sb(name, shape, dt=F32):
        t = es.enter_context(nc.sbuf_tensor(name, list(shape), dt))
        return t, Res(name, small=(int(np.prod(shape[1:])) <= 256))

    def newsem(name):
        return es.enter_context(nc.semaphore(name))

    PE = Eng("pe", nc.tensor, newsem("s_pe"))
    ACT = Eng("act", nc.scalar, newsem("s_act"))
    DVE = Eng("dve", nc.vector, newsem("s_dve"))
    POOL = Eng("pool", nc.gpsimd, newsem("s_pool"))
    SP = Eng("sp", nc.sync, newsem("s_sp"))
    _dsn = [0]
    all_ds = []

    def dsem():
        _dsn[0] += 1
        d = DSem(newsem(f"d{_dsn[0]}"))
        all_ds.append(d)
        return d

    def regroup(ds, rlist):
        for r in rlist:
            r.w = (ds.sem, ds.total)

    def barrier():
        engs = [PE, ACT, DVE, POOL, SP]
        for E in engs:
            for Fe in engs:
                if Fe is not E and Fe.cnt > 0:
                    E.wait((Fe.sem, Fe.cnt))
            for d in all_ds:
                if d.total > 0:
                    E.wait((d.sem, d.total))

    banks = []
    for i in range(8):
        t = es.enter_context(nc.psum_tensor(f"bank{i}", [128, 512], F32))
        banks.append((t, Res(f"bank{i}", psum=True)))

    ident_f, r_ident_f = sb("ident_f", [128, 128])
    ident_b, r_ident_b = sb("ident_b", [128, 128], BF16)
    ustr_f, r_ustr_f = sb("ustr_f", [128, 128])
    ustr_b, r_ustr_b = sb("ustr_b", [128, 128], BF16)
    ones_b, r_ones_b = sb("ones_b", [128, 128], BF16)
    causal_f, r_causal = sb("causal_f", [128, 128])
    ecoff, r_ecoff = sb("ecoff", [128, 32])
    flags, r_flags = sb("flags", [128, 4])
    biasB, r_biasB = sb("biasB", [128, 16, 256])
    neghalf, r_neghalf = sb("neghalf", [128, 1])
    cds = dsem()
    dma(SP, cds, [], [r_ident_f], lambda: nc.sync.dma_start(out=ident_f[:], in_=c_ident))
    dma(SP, cds, [], [r_ustr_f], lambda: nc.sync.dma_start(out=ustr_f[:], in_=c_ustrict))
    dma(SP, cds, [], [r_causal], lambda: nc.sync.dma_start(out=causal_f[:], in_=c_causal))
    dma(SP, cds, [], [r_ecoff], lambda: nc.sync.dma_start(out=ecoff[:], in_=c_ecoff))
    dma(SP, cds, [], [r_flags], lambda: nc.sync.dma_start(out=flags[:], in_=c_flags))
    dma(SP, cds, [], [r_biasB], lambda: nc.sync.dma_start(out=biasB[:], in_=attn_bias))
    regroup(cds, [r_ident_f, r_ustr_f, r_causal, r_ecoff, r_flags, r_biasB])
    op(DVE, [r_ident_f], [r_ident_b], lambda: nc.vector.tensor_copy(out=ident_b[:], in_=ident_f[:]))
    op(DVE, [r_ustr_f], [r_ustr_b], lambda: nc.vector.tensor_copy(out=ustr_b[:], in_=ustr_f[:]))
    op(DVE, [], [r_ones_b], lambda: nc.vector.memset(ones_b[:], 1.0))
    op(DVE, [], [r_neghalf], lambda: nc.vector.memset(neghalf[:], -0.5))

    RI, r_RI = sb("RI", [128, NCH, 2])
    DI, r_DI = sb("DI", [128, NCH, 2], I32)
    carryB, r_carry = sb("carryB", [128, 32])

    lnG, r_lnG = sb("lnG", [128, D])
    lnB, r_lnB = sb("lnB", [128, D])
    Wr, r_Wr = sb("Wr", [128, 8, 36])
    lds = dsem()

    RBYTES = 57344
    Rt, _ = sb("Rreg", [128, RBYTES // 4])

    def rview(off, shape, dt):
        nb = int(np.prod(shape)) * (2 if dt == BF16 else 4)
        ap = Rt[:, off // 4:(off + nb) // 4]
        if dt != F32:
            ap = ap.bitcast(dt)
        if len(shape) == 2:
            return ap.rearrange("p (a b) -> p a b", a=shape[0])
        return ap

    EOt, _ = sb("EOreg", [128, 24576 // 4])

    def aview(off, shape, dt, name, small=False):
        nb = int(np.prod(shape)) * (2 if dt == BF16 else 4)
        ap = EOt[:, off // 4:(off + nb) // 4]
        if dt != F32:
            ap = ap.bitcast(dt)
        if len(shape) == 2:
            ap = ap.rearrange("p (a b) -> p a b", a=shape[0])
        elif len(shape) == 3:
            ap = ap.rearrange("p (a b c) -> p a b c", a=shape[0], b=shape[1])
        return ap, Res(name, small=small)

    Win, r_Win = rview(0, [8, 2560], BF16), Res("Win")
    Wout, r_Wout = rview(40960, [8, D], BF16), Res("Wout")
    gBg, r_gBg = aview(12352, [512], F32, "gBg")
    gBb, r_gBb = aview(14400, [512], F32, "gBb")
    bspB, r_bspB = aview(16448, [4, 128], F32, "bspB")
    wsT, r_wsT = aview(18496, [4, 128], BF16, "wsT")
    wsp_raw, r_wsp_raw = aview(19520, [4, 128], F32, "wsp_raw")
    convw, r_convw = sb("convw", [128, 4, 3])
    qb, r_qb = sb("qb", [128, 8])
    kb2, r_kb2 = sb("kb2", [128, 2])
    vbB, r_vbB = aview(19968, [128], F32, "vbB", small=True)
    boB, r_boB = aview(20480, [D], F32, "boB")
    sinkB, r_sinkB = sb("sinkB", [128, 16])
    wds = dsem()

    Wg = [sb(f"Wg{i}", [128, 8, 512], BF16) for i in range(2)]
    Wu = [sb(f"Wu{i}", [128, 8, 512], BF16) for i in range(2)]
    Wd = [sb(f"Wd{i}", [128, 4, D], BF16) for i in range(2)]
    ewds = [dsem(), dsem()]
    ewds2 = [dsem(), dsem()]

    xt = [sb(f"xt{i}", [128, D]) for i in range(2)]
    xtds = [dsem(), dsem()]
    xT, r_xT = sb("xT", [128, 8, 128], BF16)
    uT, r_uT = aview(0, [4, 128], BF16, "uT")
    vv, r_vv = aview(1024, [512], F32, "vv")
    vn, r_vn = aview(3072, [512], BF16, "vn")
    hb, r_hb = aview(4096, [512], F32, "hb")
    zz = [aview(6144 + i * 2080, [4, 130], F32, f"zz{i}", small=True) for i in range(2)]
    acc, r_acc = aview(10304, [4, 128], F32, "acc", small=True)

    yT, r_yT = sb("yT", [128, 8, 128], BF16)
    tt, r_tt = sb("tt", [128, D])
    x1, r_x1 = tt, r_tt
    x1T, r_x1T = sb("x1T", [128, 8, 128])
    x1b, r_x1b = sb("x1b", [128, D], BF16)
    st6_, mv_, rstd_, nmr_ = ([sb(f"{n}{i}", sh) for i in range(2)] for n, sh in (("st6", [128, 12]), ("mv", [128, 2]), ("rstd", [128, 1]), ("nmr", [128, 1])))
    st6, r_st6 = st6_[0]
    mv, r_mv = mv_[0]
    rstd, r_rstd = rstd_[0]
    nmr, r_nmr = nmr_[0]
    lg, r_lg = sb("lg", [128, 36])
    rs, r_rs = sb("rs", [128, 64])
    selm, r_selm = sb("selm", [128, 32])
    top8, r_top8 = sb("top8", [128, 8])
    oh1, r_oh1 = sb("oh1", [128, 32])
    oh2, r_oh2 = sb("oh2", [128, 32])
    Aoh, r_Aoh = sb("Aoh", [128, 32], BF16)
    posf, r_posf = sb("posf", [128, 32])
    tmp32, r_tmp32 = sb("tmp32", [128, 32])
    oh1_s, r_ohs = sb("oh1_s", [128, 32])
    destf, r_destf = sb("destf", [128, 2])
    qT, r_qT = aview(0, [8, 128], BF16, "qT")
    kT, r_kT = aview(2048, [2, 2, 128], BF16, "kT")
    r_kTs = [Res("kTs0"), Res("kTs1")]
    vtok, r_vtok_ = aview(3072, [2, 128], BF16, "vtok")
    r_vts = [Res("vts0"), Res("vts1")]
    ssb_ = [aview(3584 + i * 4096, [4, 256], F32, f"ssb{i}") for i in range(2)]
    pp_ = [aview(11776 + i * 2048, [4, 256], BF16, f"pp{i}") for i in range(2)]
    r_mrow_ = [Res(f"mrow{i}", small=True) for i in range(4)]
    r_nmrow_ = [Res(f"nmrow{i}", small=True) for i in range(4)]
    r_rsum_ = [Res(f"rsum{i}", small=True) for i in range(4)]
    r_esink_ = [Res(f"esink{i}", small=True) for i in range(4)]
    r_rden_ = [Res(f"rden{i}", small=True) for i in range(4)]
    pT, r_pT = aview(15872, [8, 128], BF16, "pT")
    osb, r_osb = aview(17920, [D], BF16, "osb")
    mrow, r_mrow = sb("mrow", [128, 16])
    nmrow, r_nmrow = sb("nmrow", [128, 16])
    rsum, r_rsum = sb("rsum", [128, 16])
    esink, r_esink = sb("esink", [128, 16])
    rden, r_rden = sb("rden", [128, 16])
    y1 = [(rview(i * 4096, [D], F32), Res(f"y1_{i}")) for i in range(2)]
    y2 = [(rview(8192 + i * 4096, [D], F32), Res(f"y2_{i}")) for i in range(2)]
    gds = [dsem(), dsem()]
    xc = [(rview(16384 + i * 4096, [D], F32), Res(f"xc{i}")) for i in range(2)]
    xcds = [dsem(), dsem()]
    x2t_ = [(rview(24576 + i * 4096, [D], F32), Res(f"x2t{i}")) for i in range(2)]
    ttc_ = [(rview(32768 + i * 4096, [D], F32), Res(f"ttc{i}")) for i in range(2)]
    xs = [(rview(i * 6144, [RB, D], BF16), Res(f"xs{i}")) for i in range(2)]
    xsds = [dsem(), dsem()]
    xsT, r_xsT = rview(12288, [8, CAP], BF16), Res("xsT")
    sg = [(rview(18432 + i * 1536, [CAP], F32), Res(f"sg{i}")) for i in range(2)]
    hT, r_hT = rview(21504, [4, CAP], BF16), Res("hT")
    ys = [(rview(24576 + i * 12288, [RB, D], F32), Res(f"ys{i}")) for i in range(2)]
    ysds = [dsem(), dsem()]
    sc_ds = dsem()
    st_ds = dsem()
    r_X1 = [Res(f"X1_{c}") for c in range(NCH)]
    r_X2 = [Res(f"X2_{c}") for c in range(NCH)]
    out_toks = []

    V = nc.vector
    S = nc.scalar
    G = nc.gpsimd
    T = nc.tensor

    def layer_norm(src, r_src, dst, r_dst, gB, r_gB, bB, r_bB, par=0, bias_on_pool=False):
        (st6, r_st6), (mv, r_mv), (rstd, r_rstd), (nmr, r_nmr) = st6_[par], mv_[par], rstd_[par], nmr_[par]
        for h in range(2):
            op(DVE, [r_src], [r_st6], lambda h=h: V.bn_stats(out=st6[:, h * 6:(h + 1) * 6], in_=src[:, h * 512:(h + 1) * 512]))
        op(DVE, [r_st6], [r_mv], lambda: V.bn_aggr(out=mv[:], in_=st6[:]))
        op(DVE, [r_mv], [r_rstd], lambda: V.tensor_scalar(out=rstd[:], in0=mv[:, 1:2], scalar1=EPS, scalar2=None, op0=ALU.add))
        op(POOL, [r_rstd, r_neghalf], [r_rstd], lambda: G.tensor_tensor(out=rstd[:], in0=rstd[:], in1=neghalf[:], op=ALU.pow))
        op(DVE, [r_mv, r_rstd], [r_nmr], lambda: V.scalar_tensor_tensor(out=nmr[:], in0=mv[:, 0:1], scalar=-1.0, in1=rstd[:], op0=ALU.mult, op1=ALU.mult))
        op(ACT, [r_src, r_rstd, r_nmr], [r_dst], lambda: S.activation(out=dst[:], in_=src[:], func=AF.Identity, bias=nmr[:], scale=rstd[:]))
        op(DVE, [r_dst, r_gB], [r_dst], lambda: V.tensor_tensor(out=dst[:], in0=dst[:], in1=gB[:], op=ALU.mult))
        if bias_on_pool:
            op(POOL, [r_dst, r_bB], [r_dst], lambda: G.tensor_tensor(out=dst[:], in0=dst[:], in1=bB[:], op=ALU.add))
        else:
            op(DVE, [r_dst, r_bB], [r_dst], lambda: V.tensor_tensor(out=dst[:], in0=dst[:], in1=bB[:], op=ALU.add))

    def bcast_load(dst, r_dst, src_vec):
        dma(SP, lds, [], [r_dst], lambda: nc.sync.dma_start(out=dst, in_=src_vec.partition_broadcast(128)))

    def load_expert(l, e, part=None):
        s = e % 2
        (wg, r_wg), (wu, r_wu), (wd, r_wd) = Wg[s], Wu[s], Wd[s]
        if part in (None, 0):
            dma(POOL, ewds[s], [], [r_wg], lambda: G.dma_start(out=wg[:], in_=w_gate[l, e].rearrange("(kt p) n -> p kt n", p=128)))
            dma(POOL, ewds[s], [], [r_wu], lambda: G.dma_start(out=wu[:], in_=w_up[l, e].rearrange("(kt p) n -> p kt n", p=128)))
            regroup(ewds[s], [r_wg, r_wu])
        if part in (None, 1):
            dma(POOL, ewds2[s], [], [r_wd], lambda: G.dma_start(out=wd[:], in_=w_down[l, e].rearrange("(kt p) n -> p kt n", p=128)))

    def router_and_scatter(l, c):
        b0, r_b0 = banks[0]
        b1, r_b1 = banks[1]
        b4, r_b4 = banks[4]
        for kt in range(8):
            bt, r_bt = (b0, r_b0) if kt < 4 else (b1, r_b1)
            op(PE, [r_x1, r_ident_f], [r_bt], lambda kt=kt, bt=bt: T.transpose(out=bt[:, (kt % 4) * 128:(kt % 4 + 1) * 128], in_=x1[:, kt * 128:(kt + 1) * 128], identity=ident_f[:]))
        op(ACT, [r_b0], [r_x1T], lambda: S.copy(out=x1T[:, 0:4, :], in_=b0[:].rearrange("p (k t) -> p k t", k=4)))
        op(ACT, [r_b1], [r_x1T], lambda: S.copy(out=x1T[:, 4:8, :], in_=b1[:].rearrange("p (k t) -> p k t", k=4)))
        op(ACT, [r_x1], [r_x1b], lambda: S.copy(out=x1b[:], in_=x1[:]))
        dma(SP, st_ds, [r_x1], [r_X1[c]], lambda: nc.sync.dma_start(out=X1[c * 128:(c + 1) * 128, :], in_=x1[:]))
        for kt in range(8):
            op(PE, [r_x1T, r_Wr], [r_b4], lambda kt=kt: T.matmul(b4[:, 0:36], lhsT=x1T[:, kt, :], rhs=Wr[:, kt, :], start=(kt == 0), stop=(kt == 7)))
        op(DVE, [r_b4], [r_lg], lambda: V.tensor_copy(out=lg[:], in_=b4[:, 0:36]))
        gmax, ngmax, gsum, gp = rs[:, 0:1], rs[:, 1:2], rs[:, 2:3], rs[:, 3:4]
        ohg, negm, gex = rs[:, 4:8], rs[:, 8:12], rs[:, 12:16]
        op(DVE, [r_lg], [r_rs], lambda: V.tensor_reduce(out=gmax, in_=lg[:, 0:4], axis=AX.X, op=ALU.max))
        op(DVE, [r_rs], [r_rs], lambda: V.tensor_scalar(out=ngmax, in0=gmax, scalar1=-1.0, scalar2=None, op0=ALU.mult))
        op(ACT, [r_lg, r_rs], [r_rs], lambda: S.activation(out=gex, in_=lg[:, 0:4], func=AF.Exp, bias=ngmax, scale=1.0, accum_out=gsum))
        op(DVE, [r_rs], [r_rs], lambda: V.reciprocal(out=gp, in_=gsum))
        op(DVE, [r_lg, r_rs], [r_rs], lambda: V.tensor_scalar(out=ohg, in0=lg[:, 0:4], scalar1=gmax, scalar2=None, op0=ALU.is_equal))
        op(DVE, [r_rs], [r_rs], lambda: V.tensor_scalar(out=negm, in0=ohg, scalar1=-1.0, scalar2=-NEG, op0=ALU.add, op1=ALU.mult))
        op(DVE, [r_lg, r_rs], [r_selm], lambda: V.tensor_tensor(out=selm[:].rearrange("p (g e) -> p g e", g=4), in0=lg[:, 4:36].rearrange("p (g e) -> p g e", g=4), in1=negm.unsqueeze(2).to_broadcast([128, 4, 8]), op=ALU.add))
        op(DVE, [r_selm], [r_top8], lambda: V.max(out=top8[:], in_=selm[:]))
        op(DVE, [r_selm, r_top8], [r_oh1], lambda: V.tensor_scalar(out=oh1[:], in0=selm[:], scalar1=top8[:, 0:1], scalar2=None, op0=ALU.is_equal))
        op(DVE, [r_selm, r_top8], [r_oh2], lambda: V.tensor_scalar(out=oh2[:], in0=selm[:], scalar1=top8[:, 1:2], scalar2=None, op0=ALU.is_equal))
        op(DVE, [r_oh1, r_oh2], [r_Aoh], lambda: V.tensor_tensor(out=Aoh[:], in0=oh1[:], in1=oh2[:], op=ALU.add))
        if c < HALO:
            op(DVE, [r_Aoh, r_flags], [r_Aoh], lambda: V.tensor_scalar(out=Aoh[:], in0=Aoh[:], scalar1=flags[:, 0:1], scalar2=None, op0=ALU.mult))
        dlt, ex, w1 = rs[:, 16:17], rs[:, 17:18], rs[:, 18:19]
        op(DVE, [r_top8], [r_rs], lambda: V.tensor_tensor(out=dlt, in0=top8[:, 1:2], in1=top8[:, 0:1], op=ALU.subtract))
        op(ACT, [r_rs], [r_rs], lambda: S.activation(out=ex, in_=dlt, func=AF.Exp))
        op(DVE, [r_rs], [r_rs], lambda: V.tensor_scalar(out=ex, in0=ex, scalar1=1.0, scalar2=None, op0=ALU.add))
        op(DVE, [r_rs], [r_rs], lambda: V.reciprocal(out=w1, in_=ex))
        op(DVE, [r_rs], [r_RI], lambda: V.tensor_tensor(out=RI[:, c, 0:1], in0=w1, in1=gp, op=ALU.mult))
        op(DVE, [r_rs, r_RI], [r_RI], lambda: V.tensor_tensor(out=RI[:, c, 1:2], in0=gp, in1=RI[:, c, 0:1], op=ALU.subtract))
        op(PE, [r_ustr_b, r_Aoh], [r_b4], lambda: T.matmul(b4[:, 64:96], lhsT=ustr_b[:], rhs=Aoh[:], start=True, stop=True))
        op(PE, [r_ones_b, r_Aoh], [r_b4], lambda: T.matmul(b4[:, 128:160], lhsT=ones_b[:], rhs=Aoh[:], start=True, stop=True))
        op(DVE, [r_b4, r_carry], [r_posf], lambda: V.tensor_tensor(out=posf[:], in0=b4[:, 64:96], in1=carryB[:], op=ALU.add))
        op(DVE, [r_b4, r_carry], [r_carry], lambda: V.tensor_tensor(out=carryB[:], in0=b4[:, 128:160], in1=carryB[:], op=ALU.add))
        op(DVE, [r_posf], [r_tmp32], lambda: V.tensor_scalar(out=tmp32[:], in0=posf[:], scalar1=float(CAP) - 0.5, scalar2=None, op0=ALU.is_gt))
        if c < HALO:
            op(DVE, [r_tmp32, r_flags], [r_tmp32], lambda: V.tensor_scalar(out=tmp32[:], in0=tmp32[:], scalar1=flags[:, 3:4], scalar2=None, op0=ALU.max))
        op(DVE, [r_posf, r_ecoff], [r_posf], lambda: V.tensor_tensor(out=posf[:], in0=posf[:], in1=ecoff[:], op=ALU.add))
        op(DVE, [r_posf, r_flags], [r_ohs], lambda: V.tensor_scalar(out=oh1_s[:], in0=posf[:], scalar1=flags[:, 2:3], scalar2=None, op0=ALU.subtract))
        op(DVE, [r_ohs, r_tmp32], [r_tmp32], lambda: V.tensor_tensor(out=tmp32[:], in0=oh1_s[:], in1=tmp32[:], op=ALU.mult))
        op(DVE, [r_posf, r_tmp32], [r_posf], lambda: V.tensor_tensor(out=posf[:], in0=posf[:], in1=tmp32[:], op=ALU.subtract))
        op(DVE, [r_posf, r_oh1], [r_tmp32], lambda: V.tensor_tensor(out=tmp32[:], in0=posf[:], in1=oh1[:], op=ALU.mult))
        op(DVE, [r_tmp32], [r_destf], lambda: V.tensor_reduce(out=destf[:, 0:1], in_=tmp32[:], axis=AX.X, op=ALU.add))
        op(DVE, [r_posf, r_oh2], [r_tmp32], lambda: V.tensor_tensor(out=tmp32[:], in0=posf[:], in1=oh2[:], op=ALU.mult))
        op(DVE, [r_tmp32], [r_destf], lambda: V.tensor_reduce(out=destf[:, 1:2], in_=tmp32[:], axis=AX.X, op=ALU.add))
        op(DVE, [r_destf], [r_DI], lambda: V.tensor_copy(out=DI[:, c, :], in_=destf[:]))
        for k in range(2):
            dma(POOL, sc_ds, [r_x1b, r_DI], [], lambda k=k: G.indirect_dma_start(
                out=XS, out_offset=bass.IndirectOffsetOnAxis(ap=DI[:, c, k:k + 1], axis=0),
                in_=x1b[:], in_offset=None))

    def transposes_x(src, r_src):
        b0, r_b0 = banks[0]
        b1, r_b1 = banks[1]
        for kt in range(8):
            bt, r_bt = (b0, r_b0) if kt < 4 else (b1, r_b1)
            op(PE, [r_src, r_ident_f], [r_bt], lambda kt=kt, bt=bt: T.transpose(out=bt[:, (kt % 4) * 128:(kt % 4 + 1) * 128], in_=src[:, kt * 128:(kt + 1) * 128], identity=ident_f[:]))
        op(ACT, [r_b0], [r_xT], lambda: S.copy(out=xT[:, 0:4, :], in_=b0[:].rearrange("p (k t) -> p k t", k=4)))
        op(ACT, [r_b1], [r_xT], lambda: S.copy(out=xT[:, 4:8, :], in_=b1[:].rearrange("p (k t) -> p k t", k=4)))

    def mixer_even(l, c, src, r_src, part):
        b2, r_b2 = banks[2]
        b3, r_b3 = banks[3]
        b4, r_b4 = banks[4]
        if part == 0:
            for j in range(4):
                for kt in range(8):
                    op(PE, [r_Win, r_xT], [r_b2], lambda j=j, kt=kt: T.matmul(b2[:, j * 128:(j + 1) * 128], lhsT=Win[:, kt, j * 128:(j + 1) * 128], rhs=xT[:, kt, :], start=(kt == 0), stop=(kt == 7)))
            for kt in range(8):
                op(PE, [r_Win, r_xT], [r_b3], lambda kt=kt: T.matmul(b3[:, :], lhsT=xT[:, kt, :], rhs=Win[:, kt, 512:1024], start=(kt == 0), stop=(kt == 7)))
            for j in range(12):
                bt, r_bt = banks[5 + j // 4]
                for kt in range(8):
                    op(PE, [r_Win, r_xT], [r_bt], lambda j=j, kt=kt, bt=bt: T.matmul(bt[:, (j % 4) * 128:(j % 4 + 1) * 128], lhsT=Win[:, kt, 1024 + j * 128:1024 + (j + 1) * 128], rhs=xT[:, kt, :], start=(kt == 0), stop=(kt == 7)))
            op(ACT, [r_b2], [r_uT], lambda: S.activation(out=uT[:].rearrange("p g t -> p (g t)"), in_=b2[:], func=AF.Gelu))
            op(ACT, [r_b3], [r_vv], lambda: S.activation(out=vv[:], in_=b3[:], func=AF.Gelu))
            b7, r_b7 = banks[7]
            op(ACT, [r_b7], [r_hb], lambda: S.copy(out=hb[:], in_=b7[:]))
            return
        op(DVE, [r_vv], [r_st6], lambda: V.bn_stats(out=st6[:, 0:6], in_=vv[:]))
        op(DVE, [r_st6], [r_mv], lambda: V.bn_aggr(out=mv[:], in_=st6[:, 0:6]))
        op(DVE, [r_mv], [r_rstd], lambda: V.tensor_scalar(out=rstd[:], in0=mv[:, 1:2], scalar1=EPS, scalar2=None, op0=ALU.add))
        op(POOL, [r_rstd, r_neghalf], [r_rstd], lambda: G.tensor_tensor(out=rstd[:], in0=rstd[:], in1=neghalf[:], op=ALU.pow))
        op(DVE, [r_vv, r_mv, r_rstd], [r_vv], lambda: V.tensor_scalar(out=vv[:], in0=vv[:], scalar1=mv[:, 0:1], scalar2=rstd[:], op0=ALU.subtract, op1=ALU.mult))
        op(DVE, [r_vv, r_gBg], [r_vv], lambda: V.tensor_tensor(out=vv[:], in0=vv[:], in1=gBg[:], op=ALU.mult))
        op(DVE, [r_vv, r_gBb], [r_vn], lambda: V.tensor_tensor(out=vn[:], in0=vv[:], in1=gBb[:], op=ALU.add))
        for g in range(4):
            op(PE, [r_vn, r_wsT], [r_b4], lambda g=g: T.matmul(b4[:, g * 128:(g + 1) * 128], lhsT=vn[:, g * 128:(g + 1) * 128], rhs=wsT[:, g, :], start=True, stop=True))
        op(DVE, [r_b4, r_bspB], [r_acc], lambda: V.tensor_tensor(out=acc[:].rearrange("p g t -> p (g t)"), in0=b4[:], in1=bspB[:].rearrange("p g t -> p (g t)"), op=ALU.add))
        op(DVE, [r_acc, r_uT], [r_yT], lambda: V.tensor_tensor(out=yT[:, 0:4, :], in0=acc[:], in1=uT[:], op=ALU.mult))
        b5, r_b5 = banks[5]
        b6, r_b6 = banks[6]
        b7, r_b7 = banks[7]
        (zc, r_zc), (zp, r_zp) = zz[c % 2], zz[(c + 1) % 2]
        op(DVE, [r_b6, r_hb], [r_zc], lambda: V.tensor_tensor(out=zc[:, :, 2:130], in0=b6[:].rearrange("p (g t) -> p g t", g=4), in1=hb[:].rearrange("p (g t) -> p g t", g=4), op=ALU.mult))
        if c == 0:
            op(DVE, [], [r_zc], lambda: V.memset(zc[:, :, 0:2], 0.0))
        elif c == HALO:
            op(DVE, [r_zp, r_flags], [r_zc], lambda: V.tensor_scalar(out=zc[:, :, 0:2], in0=zp[:, :, 128:130], scalar1=flags[:, 0:1], scalar2=None, op0=ALU.mult))
        else:
            op(DVE, [r_zp], [r_zc], lambda: V.tensor_copy(out=zc[:, :, 0:2], in_=zp[:, :, 128:130]))
        for g in range(4):
            op(DVE, [r_zc, r_convw], [r_acc], lambda g=g: V.tensor_scalar(out=acc[:, g, :], in0=zc[:, g, 0:128], scalar1=convw[:, g, 0:1], scalar2=None, op0=ALU.mult))
            op(DVE, [r_zc, r_convw, r_acc], [r_acc], lambda g=g: V.scalar_tensor_tensor(out=acc[:, g, :], in0=zc[:, g, 1:129], scalar=convw[:, g, 1:2], in1=acc[:, g, :], op0=ALU.mult, op1=ALU.add))
            op(DVE, [r_zc, r_convw, r_acc], [r_acc], lambda g=g: V.scalar_tensor_tensor(out=acc[:, g, :], in0=zc[:, g, 2:130], scalar=convw[:, g, 2:3], in1=acc[:, g, :], op0=ALU.mult, op1=ALU.add))
        op(DVE, [r_b5, r_acc], [r_yT], lambda: V.tensor_tensor(out=yT[:, 4:8, :], in0=b5[:].rearrange("p (g t) -> p g t", g=4), in1=acc[:], op=ALU.mult))
        for h in range(2):
            bt, r_bt = banks[2 + h]
            for kt in range(8):
                op(PE, [r_yT, r_Wout], [r_bt], lambda h=h, kt=kt, bt=bt: T.matmul(bt[:, :], lhsT=yT[:, kt, :], rhs=Wout[:, kt, h * 512:(h + 1) * 512], start=(kt == 0), stop=(kt == 7)))
        for h in range(2):
            bt, r_bt = banks[2 + h]
            op(DVE, [r_src, r_bt], [r_tt], lambda h=h, bt=bt: V.scalar_tensor_tensor(out=tt[:, h * 512:(h + 1) * 512], in0=src[:, h * 512:(h + 1) * 512], scalar=ALPHA, in1=bt[:, :], op0=ALU.mult, op1=ALU.add))

    def mixer_odd(l, c, src, r_src, part):
        b2, r_b2 = banks[2]
        b3, r_b3 = banks[3]
        b4, r_b4 = banks[4]
        sl, ps = c % 2, (c + 1) % 2
        if part == 0:
            for j in range(8):
                bt, r_bt = banks[2 + j // 4]
                for kt in range(8):
                    op(PE, [r_Win, r_xT], [r_bt], lambda j=j, kt=kt, bt=bt: T.matmul(bt[:, (j % 4) * 128:(j % 4 + 1) * 128], lhsT=Win[:, kt, j * 128:(j + 1) * 128], rhs=xT[:, kt, :], start=(kt == 0), stop=(kt == 7)))
            for kv in range(2):
                for kt in range(8):
                    op(PE, [r_Win, r_xT], [r_b4], lambda kv=kv, kt=kt: T.matmul(b4[:, kv * 128:(kv + 1) * 128], lhsT=Win[:, kt, 1024 + kv * 128:1024 + (kv + 1) * 128], rhs=xT[:, kt, :], start=(kt == 0), stop=(kt == 7)))
            for kt in range(8):
                op(PE, [r_Win, r_xT], [r_b4], lambda kt=kt: T.matmul(b4[:, 256:384], lhsT=xT[:, kt, :], rhs=Win[:, kt, 1280:1408], start=(kt == 0), stop=(kt == 7)))
            for j in range(8):
                bt, r_bt = banks[2 + j // 4]
                op(ACT, [r_bt, r_qb], [r_qT], lambda j=j, bt=bt: S.activation(out=qT[:, j, :], in_=bt[:, (j % 4) * 128:(j % 4 + 1) * 128], func=AF.Identity, bias=qb[:, j:j + 1], scale=0.125))
            for kv in range(2):
                op(ACT, [r_b4, r_kb2], [r_kTs[sl]], lambda kv=kv: S.activation(out=kT[:, kv, sl, :], in_=b4[:, kv * 128:(kv + 1) * 128], func=AF.Identity, bias=kb2[:, kv:kv + 1], scale=1.0))
            op(DVE, [r_b4, r_vbB], [r_vts[sl]], lambda: V.tensor_tensor(out=vtok[:, sl, :], in0=b4[:, 256:384], in1=vbB[:], op=ALU.add))
            if c == 0:
                for kv in range(2):
                    op(DVE, [], [r_kTs[ps]], lambda kv=kv: V.memset(kT[:, kv, ps, :], 0.0))
                op(DVE, [], [r_vts[ps]], lambda: V.memset(vtok[:, ps, :], 0.0))
            return
        b7, r_b7 = banks[7]
        b7b = b7[:].bitcast(BF16)

        def att_s1(hg):
            sset = hg % 2
            (ssbx, r_ssbx), (ppx, r_ppx) = ssb_[sset], pp_[sset]
            (sb0, r_sb0), (sb1, r_sb1) = (banks[5], banks[6]) if sset == 0 else (banks[2], banks[3])
            for hh in range(4):
                h = hg * 4 + hh
                bt, r_bt = (sb0, r_sb0) if hh % 2 == 0 else (sb1, r_sb1)
                pr = (h % 2) * 64
                kv = h // 8
                for blk, slot in ((0, ps), (1, sl)):
                    op(PE, [r_qT, r_kTs[slot]], [r_bt], lambda h=h, bt=bt, pr=pr, kv=kv, blk=blk, slot=slot, hh=hh: T.matmul(
                        bt[:, (hh // 2) * 256 + blk * 128:(hh // 2) * 256 + (blk + 1) * 128],
                        lhsT=qT[pr:pr + 64, h // 2, :], rhs=kT[pr:pr + 64, kv, slot, :], start=True, stop=True))
            for par in range(2):
                bt, r_bt = (sb0, r_sb0) if par == 0 else (sb1, r_sb1)
                for i2 in range(2):
                    hh = par + 2 * i2
                    op(DVE, [r_bt, r_biasB], [r_ssbx], lambda bt=bt, hh=hh, i2=i2: V.tensor_tensor(out=ssbx[:, hh, :], in0=bt[:, i2 * 256:(i2 + 1) * 256], in1=biasB[:, hg * 4 + hh, :], op=ALU.add))
            if c == HALO:
                op(DVE, [r_ssbx, r_flags], [r_ssbx], lambda: V.tensor_scalar(out=ssbx[:, :, 0:128], in0=ssbx[:, :, 0:128], scalar1=flags[:, 1:2], scalar2=None, op0=ALU.add))
            if c == 0:
                op(DVE, [r_ssbx], [r_ssbx], lambda: V.tensor_scalar(out=ssbx[:, :, 0:128], in0=ssbx[:, :, 0:128], scalar1=NEG, scalar2=None, op0=ALU.add))
            h4 = slice(hg * 4, hg * 4 + 4)
            r_m, r_nm, r_rs_ = r_mrow_[hg], r_nmrow_[hg], r_rsum_[hg]
            op(DVE, [r_ssbx], [r_m], lambda: V.tensor_reduce(out=mrow[:, h4], in_=ssbx[:], axis=AX.X, op=ALU.max))
            op(DVE, [r_m, r_sinkB], [r_m], lambda: V.tensor_tensor(out=mrow[:, h4], in0=mrow[:, h4], in1=sinkB[:, h4], op=ALU.max))
            op(DVE, [r_m], [r_nm], lambda: V.tensor_scalar(out=nmrow[:, h4], in0=mrow[:, h4], scalar1=-1.0, scalar2=None, op0=ALU.mult))
            for hh in range(4):
                h = hg * 4 + hh
                op(ACT, [r_ssbx, r_nm], [r_ppx, r_rs_], lambda h=h, hh=hh: S.activation(out=ppx[:, hh, :], in_=ssbx[:, hh, :], func=AF.Exp, bias=nmrow[:, h:h + 1], scale=1.0, accum_out=rsum[:, h:h + 1]))

        def att_s2(hg):
            sset = hg % 2
            (ppx, r_ppx) = pp_[sset]
            h4 = slice(hg * 4, hg * 4 + 4)
            r_m, r_rs_, r_es, r_rd = r_mrow_[hg], r_rsum_[hg], r_esink_[hg], r_rden_[hg]
            for hh in range(4):
                for blk in range(2):
                    i = hh * 2 + blk
                    op(PE, [r_ppx, r_ident_b], [r_b7], lambda hh=hh, blk=blk, i=i: T.transpose(out=b7b[:, i * 128:(i + 1) * 128], in_=ppx[:, hh, blk * 128:(blk + 1) * 128], identity=ident_b[:]))
            op(ACT, [r_b7], [r_pT], lambda: S.copy(out=pT[:].rearrange("p i q -> p (i q)"), in_=b7b))
            for hh in range(4):
                h = hg * 4 + hh
                kv = h // 8
                for blk, slot in ((0, ps), (1, sl)):
                    op(PE, [r_pT, r_vts[slot]], [r_b4], lambda hh=hh, kv=kv, blk=blk, slot=slot: T.matmul(
                        b4[:, hh * 64:(hh + 1) * 64], lhsT=pT[:, hh * 2 + blk, :], rhs=vtok[:, slot, kv * 64:(kv + 1) * 64], start=(blk == 0), stop=(blk == 1)))
            op(DVE, [r_sinkB, r_m], [r_es], lambda: V.tensor_tensor(out=esink[:, h4], in0=sinkB[:, h4], in1=mrow[:, h4], op=ALU.subtract))
            op(ACT, [r_es], [r_es], lambda: S.activation(out=esink[:, h4], in_=esink[:, h4], func=AF.Exp))
            op(DVE, [r_es, r_rs_], [r_es], lambda: V.tensor_tensor(out=esink[:, h4], in0=esink[:, h4], in1=rsum[:, h4], op=ALU.add))
            op(DVE, [r_es], [r_rd], lambda: V.reciprocal(out=rden[:, h4], in_=esink[:, h4]))
            op(DVE, [r_b4, r_rd], [r_osb], lambda: V.tensor_tensor(out=osb[:, hg * 256:(hg + 1) * 256].rearrange("p (h d) -> p h d", h=4), in0=b4[:, 0:256].rearrange("p (h d) -> p h d", h=4), in1=rden[:, h4].unsqueeze(2).to_broadcast([128, 4, 64]), op=ALU.mult))

        att_s1(0)
        for hg in range(4):
            if hg + 1 < 4:
                att_s1(hg + 1)
            att_s2(hg)
        b7, r_b7 = banks[7]
        b7b = b7[:].bitcast(BF16)
        for kt in range(8):
            op(PE, [r_osb, r_ident_b], [r_b7], lambda kt=kt: T.transpose(out=b7b[:, kt * 128:(kt + 1) * 128], in_=osb[:, kt * 128:(kt + 1) * 128], identity=ident_b[:]))
        op(ACT, [r_b7], [r_yT], lambda: S.copy(out=yT[:].rearrange("p k t -> p (k t)"), in_=b7b))
        for h in range(2):
            bt, r_bt = banks[2 + h]
            for kt in range(8):
                op(PE, [r_yT, r_Wout], [r_bt], lambda h=h, kt=kt, bt=bt: T.matmul(bt[:, :], lhsT=yT[:, kt, :], rhs=Wout[:, kt, h * 512:(h + 1) * 512], start=(kt == 0), stop=(kt == 7)))
        for h in range(2):
            bt, r_bt = banks[2 + h]
            op(DVE, [r_src, r_bt], [r_tt], lambda h=h, bt=bt: V.scalar_tensor_tensor(out=tt[:, h * 512:(h + 1) * 512], in0=src[:, h * 512:(h + 1) * 512], scalar=ALPHA, in1=bt[:, :], op0=ALU.mult, op1=ALU.add))
        op(POOL, [r_tt, r_boB], [r_tt], lambda: G.tensor_tensor(out=tt[:], in0=tt[:], in1=boB[:], op=ALU.add))

    def load_layer_consts(l):
        i = l // 2
        bcast_load(lnG[:], r_lnG, ln1_g[l])
        bcast_load(lnB[:], r_lnB, ln1_b[l])
        dma(SP, lds, [], [r_Wr], lambda: nc.sync.dma_start(out=Wr[:, :, 0:4], in_=router_group[l].rearrange("(kt p) n -> p kt n", p=128)))
        dma(SP, lds, [], [r_Wr], lambda: nc.sync.dma_start(out=Wr[:, :, 4:36], in_=router_expert[l].rearrange("(kt p) n -> p kt n", p=128)))
        op(DVE, [], [r_carry], lambda: V.memset(carryB[:], 0.0))
        if l % 2 == 0:
            for h in range(2):
                dma(POOL, wds, [], [r_Win], lambda h=h: G.dma_start(out=Win[:, :, h * 1280:(h + 1) * 1280], in_=mix_w_in[i][:, h * 1280:(h + 1) * 1280].rearrange("(kt p) n -> p kt n", p=128)))
            dma(POOL, wds, [], [r_Wout], lambda: G.dma_start(out=Wout[:], in_=mix_w_out[i].rearrange("(kt p) n -> p kt n", p=128)))
            bcast_load(gBg[:], r_gBg, gmlp_ln_g[i])
            bcast_load(gBb[:], r_gBb, gmlp_ln_b[i])
            bcast_load(bspB[:].rearrange("p g t -> p (g t)"), r_bspB, gmlp_b_sp[i].rearrange("g t -> (g t)"))
            dma(SP, lds, [], [r_wsp_raw], lambda: nc.sync.dma_start(out=wsp_raw[:], in_=gmlp_w_sp[i].rearrange("g i j -> i g j")))
            for k in range(3):
                dma(SP, lds, [], [r_convw], lambda k=k: nc.sync.dma_start(out=convw[:, :, k:k + 1], in_=conv_w[i, k].rearrange("(g p o) -> p g o", p=128, o=1), allow_slow_non_contiguous=True))
            regroup(lds, [r_lnG, r_lnB, r_Wr, r_gBg, r_gBb, r_bspB, r_wsp_raw, r_convw])
            regroup(wds, [r_Win, r_Wout])
            b4, r_b4 = banks[4]
            for g in range(4):
                op(PE, [r_wsp_raw, r_ident_f], [r_b4], lambda g=g: T.transpose(out=b4[:, g * 128:(g + 1) * 128], in_=wsp_raw[:, g, :], identity=ident_f[:]))
            op(DVE, [r_b4, r_causal], [r_wsT], lambda: V.tensor_tensor(out=wsT[:], in0=b4[:].rearrange("p (g i) -> p g i", g=4), in1=causal_f[:].unsqueeze(1).to_broadcast([128, 4, 128]), op=ALU.mult))
        else:
            wq = attn_w_qkv[i]
            dma(POOL, wds, [], [r_Win], lambda: G.dma_start(out=Win[:, :, 0:1024], in_=wq[:, 0:1024].rearrange("(kt p) n -> p kt n", p=128)))
            for kv in range(2):
                for dup in range(2):
                    dma(POOL, wds, [], [r_Win], lambda kv=kv, dup=dup: G.dma_start(out=Win[:, :, 1024 + kv * 128 + dup * 64:1024 + kv * 128 + (dup + 1) * 64], in_=wq[:, 1024 + kv * 64:1024 + (kv + 1) * 64].rearrange("(kt p) n -> p kt n", p=128)))
            dma(POOL, wds, [], [r_Win], lambda: G.dma_start(out=Win[:, :, 1280:1408], in_=wq[:, 1152:1280].rearrange("(kt p) n -> p kt n", p=128)))
            dma(POOL, wds, [], [r_Wout], lambda: G.dma_start(out=Wout[:], in_=attn_w_o[i].rearrange("(kt p) n -> p kt n", p=128)))
            dma(SP, lds, [], [r_qb], lambda: nc.sync.dma_start(out=qb[:].unsqueeze(2), in_=attn_b_qkv[i, 0:1024].rearrange("(j p o) -> p j o", p=128, o=1), allow_slow_non_contiguous=True))
            for kv in range(2):
                for dup in range(2):
                    dma(SP, lds, [], [r_kb2], lambda kv=kv, dup=dup: nc.sync.dma_start(out=kb2[dup * 64:(dup + 1) * 64, kv:kv + 1], in_=attn_b_qkv[i, 1024 + kv * 64:1024 + (kv + 1) * 64].rearrange("(p o) -> p o", o=1), allow_slow_non_contiguous=True))
            bcast_load(vbB[:], r_vbB, attn_b_qkv[i, 1152:1280])
            bcast_load(boB[:], r_boB, attn_b_o[i])
            bcast_load(sinkB[:], r_sinkB, attn_sinks[i])
            regroup(lds, [r_lnG, r_lnB, r_Wr, r_qb, r_kb2, r_vbB, r_boB, r_sinkB])
            regroup(wds, [r_Win, r_Wout])
            op(DVE, [r_qb], [r_qb], lambda: V.tensor_scalar(out=qb[:], in0=qb[:], scalar1=0.125, scalar2=None, op0=ALU.mult))

    def phase_M(l):
        load_layer_consts(l)
        load_expert(l, 0)
        load_expert(l, 1)
        srcD = x_in if l == 0 else X2
        r_srcD = [Res(f"xin{c}") for c in range(NCH)] if l == 0 else r_X2

        def issue_load(c):
            t, r = xt[c % 2]
            dma(SP, xtds[c % 2], [r_srcD[c]], [r], lambda: nc.sync.dma_start(out=t[:], in_=srcD[c * 128:(c + 1) * 128, :]))

        mixer = mixer_even if l % 2 == 0 else mixer_odd
        issue_load(0)
        transposes_x(*xt[0])
        mixer(l, 0, *xt[0], 0)
        for c in range(NCH):
            if c + 1 < NCH:
                issue_load(c + 1)
            src, r_src = xt[c % 2]
            mixer(l, c, src, r_src, 1)
            if c + 1 < NCH:
                transposes_x(*xt[(c + 1) % 2])
            layer_norm(tt, r_tt, x1, r_x1, lnG, r_lnG, lnB, r_lnB)
            if c + 1 < NCH:
                mixer(l, c + 1, *xt[(c + 1) % 2], 0)
            router_and_scatter(l, c)

    def phase_E(l):
        sc_tok = (sc_ds.sem, sc_ds.total)

        def load_xs(e):
            (xst, r_xst) = xs[e % 2]
            dma(SP, xsds[e % 2], [], [r_xst], lambda: nc.sync.dma_start(out=xst[:], in_=XS[e * CAP:(e + 1) * CAP, :].rearrange("(r p) d -> p r d", p=128)), extra=[sc_tok])

        for e in range(NEXP):
            s = e % 2
            (wg, r_wg), (wu, r_wu), (wd, r_wd) = Wg[s], Wu[s], Wd[s]
            (xst, r_xst) = xs[s]
            if e == 0:
                load_xs(0)
            if e + 1 < NEXP:
                load_xs(e + 1)
            for rb in range(RB):
                bt, r_bt = banks[rb % 2]
                btb = bt[:].bitcast(BF16)
                for kt in range(8):
                    op(PE, [r_xst, r_ident_b], [r_bt], lambda rb=rb, kt=kt, btb=btb: T.transpose(out=btb[:, kt * 128:(kt + 1) * 128], in_=xst[:, rb, kt * 128:(kt + 1) * 128], identity=ident_b[:]))
                if rb % 2 == 0:
                    op(ACT, [r_bt], [r_xsT], lambda rb=rb, btb=btb: S.copy(out=xsT[:, :, rb * 128:(rb + 1) * 128], in_=btb.rearrange("p (k t) -> p k t", k=8)))
                else:
                    op(DVE, [r_bt], [r_xsT], lambda rb=rb, btb=btb: V.tensor_copy(out=xsT[:, :, rb * 128:(rb + 1) * 128], in_=btb.rearrange("p (k t) -> p k t", k=8)))
            for hc in range(4):
                bg, r_bg = banks[2 + (hc % 2) * 2]
                bu, r_bu = banks[3 + (hc % 2) * 2]
                for kt in range(8):
                    op(PE, [r_wg, r_xsT], [r_bg], lambda hc=hc, kt=kt, bg=bg: T.matmul(bg[:, 0:CAP], lhsT=wg[:, kt, hc * 128:(hc + 1) * 128], rhs=xsT[:, kt, :], start=(kt == 0), stop=(kt == 7)))
                for kt in range(8):
                    op(PE, [r_wu, r_xsT], [r_bu], lambda hc=hc, kt=kt, bu=bu: T.matmul(bu[:, 0:CAP], lhsT=wu[:, kt, hc * 128:(hc + 1) * 128], rhs=xsT[:, kt, :], start=(kt == 0), stop=(kt == 7)))
                sgt, r_sgt = sg[hc % 2]
                op(ACT, [r_bg], [r_sgt], lambda bg=bg, sgt=sgt: S.activation(out=sgt[:], in_=bg[:, 0:CAP], func=AF.Silu))
                op(DVE, [r_bu, r_sgt], [r_hT], lambda hc=hc, bu=bu, sgt=sgt: V.tensor_tensor(out=hT[:, hc, :], in0=bu[:, 0:CAP], in1=sgt[:], op=ALU.mult))
            if e + 2 < NEXP:
                load_expert(l, e + 2, part=0)
            yst, r_yst = ys[s]
            for rb in range(RB):
                for half in range(2):
                    bt, r_bt = banks[6 + half]
                    for hc in range(4):
                        op(PE, [r_hT, r_wd], [r_bt], lambda rb=rb, half=half, hc=hc, bt=bt: T.matmul(bt[:, :], lhsT=hT[:, hc, rb * 128:(rb + 1) * 128], rhs=wd[:, hc, half * 512:(half + 1) * 512], start=(hc == 0), stop=(hc == 3)))
                    if half == 0:
                        op(ACT, [r_bt], [r_yst], lambda rb=rb, bt=bt: S.copy(out=yst[:, rb, 0:512], in_=bt[:, :]))
                    else:
                        op(DVE, [r_bt], [r_yst], lambda rb=rb, bt=bt: V.tensor_copy(out=yst[:, rb, 512:1024], in_=bt[:, :]))
            dma(SP, ysds[s], [r_yst], [], lambda: nc.sync.dma_start(out=YS[e * CAP:(e + 1) * CAP, :].rearrange("(r p) d -> p r d", p=128), in_=yst[:]))
            if e + 2 < NEXP:
                load_expert(l, e + 2, part=1)

    def phase_C(l, last):
        ys_toks = [(d.sem, d.total) for d in ysds]

        def issue(c):
            s = c % 2
            (a, r_a), (b, r_b), (xx, r_xx) = y1[s], y2[s], xc[s]
            dma(POOL, gds[s], [r_DI], [r_a], lambda: G.indirect_dma_start(out=a[:], out_offset=None, in_=YS, in_offset=bass.IndirectOffsetOnAxis(ap=DI[:, c, 0:1], axis=0)), extra=ys_toks)
            dma(POOL, gds[s], [r_DI], [r_b], lambda: G.indirect_dma_start(out=b[:], out_offset=None, in_=YS, in_offset=bass.IndirectOffsetOnAxis(ap=DI[:, c, 1:2], axis=0)), extra=ys_toks)
            regroup(gds[s], [r_a, r_b])
            dma(SP, xcds[s], [r_X1[c]], [r_xx], lambda: nc.sync.dma_start(out=xx[:], in_=X1[c * 128:(c + 1) * 128, :]))

        c0 = HALO if last else 0
        issue(c0)
        for c in range(c0, NCH):
            if c + 1 < NCH:
                issue(c + 1)
            s = c % 2
            (a, r_a), (b, r_b), (xx, r_xx) = y1[s], y2[s], xc[s]
            op(ACT, [r_a, r_RI], [r_a], lambda: S.activation(out=a[:], in_=a[:], func=AF.Identity, scale=RI[:, c, 0:1]))
            op(DVE, [r_b, r_RI, r_a], [r_a], lambda: V.scalar_tensor_tensor(out=a[:], in0=b[:], scalar=RI[:, c, 1:2], in1=a[:], op0=ALU.mult, op1=ALU.add))
            (ttc, r_ttc), (x2t, r_x2t) = ttc_[s], x2t_[s]
            op(DVE, [r_xx, r_a], [r_ttc], lambda: V.scalar_tensor_tensor(out=ttc[:], in0=xx[:], scalar=ALPHA, in1=a[:], op0=ALU.mult, op1=ALU.add))
            layer_norm(ttc, r_ttc, x2t, r_x2t, lnG, r_lnG, lnB, r_lnB, par=s, bias_on_pool=True)
            if dbg and HALO <= c < HALO + 8:
                dma(SP, st_ds, [r_x2t], [], lambda: nc.sync.dma_start(out=dbg_out[l, (c - HALO) * 128:(c - HALO + 1) * 128, :], in_=x2t[:]))
            if last:
                tok = dma(SP, st_ds, [r_x2t], [], lambda: nc.sync.dma_start(out=out[(c - HALO) * 128:(c - HALO + 1) * 128, :], in_=x2t[:]))
                out_toks.append(tok)
            else:
                dma(SP, st_ds, [r_x2t], [r_X2[c]], lambda: nc.sync.dma_start(out=X2[c * 128:(c + 1) * 128, :], in_=x2t[:]))

    op(DVE, [], [r_x1T], lambda: V.memset(x1T[:], 0.0))
    dma(SP, st_ds, [r_x1T], [], lambda: nc.sync.dma_start(out=YS[XS_ROWS:XS_ROWS + 128, :], in_=x1T[:].rearrange("p k t -> p (k t)")))
    for l in range(nlayers):
        phase_M(l)
        barrier()
        phase_E(l)
        barrier()
        bcast_load(lnG[:], r_lnG, ln2_g[l])
        bcast_load(lnB[:], r_lnB, ln2_b[l])
        regroup(lds, [r_lnG, r_lnB])
        phase_C(l, l == nlayers - 1)
        barrier()

    SP.wait((st_ds.sem, st_ds.total))
    nc.sync.wait_ge(st_ds.sem, st_ds.total)
    es.close()
    return nc


def _t5_bucket(rel):
    n = np.maximum(rel, 0)
    max_exact = 16
    nf = np.maximum(n, 1).astype(np.float32)
    large = max_exact + (np.log(nf / max_exact) / np.log(128 / max_exact) * (32 - max_exact)).astype(np.int32)
    large = np.minimum(large, 31)
    return np.where(n < max_exact, n, large)


def _consts(rel_bias_table):
    a = np.arange(128)[:, None]
    c = np.arange(256)[None, :]
    rel = a + 128 - c
    idx = _t5_bucket(rel)
    bias = np.asarray(rel_bias_table, np.float32)[idx]
    bias = np.ascontiguousarray(np.transpose(bias, (0, 2, 1)))
    win = (rel >= 0) & (rel < 128)
    bias[:, :, :][np.broadcast_to(~win[:, None, :], bias.shape)] = NEG
    ident = np.eye(128, dtype=np.float32)
    ustrict = np.triu(np.ones((128, 128), np.float32), 1)
    causal = np.triu(np.ones((128, 128), np.float32), 0)
    ecoff = np.broadcast_to((np.arange(32, dtype=np.float32) * CAP)[None, :], (128, 32)).copy()
    return bias, ident, ustrict, causal, ecoff


_CACHE = {}


def kernel(x, rel_bias_table, mix_w_in, gmlp_ln_g, gmlp_ln_b, gmlp_w_spatial, gmlp_b_spatial,
           conv_w, mix_w_out, attn_w_qkv, attn_b_qkv, attn_sinks, attn_w_o, attn_b_o,
           ln1_g, ln1_b, ln2_g, ln2_b, router_group, router_expert,
           expert_w_gate, expert_w_up, expert_w_down, _nlayers=DEPTH, _prep_only=False):
    f = lambda a: np.ascontiguousarray(np.asarray(a, dtype=np.float32))
    x = f(x)
    bias, ident, ustrict, causal, ecoff = _consts(rel_bias_table)
    shared = {
        "mix_w_in": f(mix_w_in), "mix_w_out": f(mix_w_out), "gmlp_ln_g": f(gmlp_ln_g),
        "gmlp_ln_b": f(gmlp_ln_b), "gmlp_w_spatial": f(gmlp_w_spatial),
        "gmlp_b_spatial": f(gmlp_b_spatial), "conv_w": f(conv_w),
        "attn_w_qkv": f(attn_w_qkv), "attn_b_qkv": f(attn_b_qkv), "attn_sinks": f(attn_sinks),
        "attn_w_o": f(attn_w_o), "attn_b_o": f(attn_b_o),
        "ln1_g": f(ln1_g), "ln1_b": f(ln1_b), "ln2_g": f(ln2_g), "ln2_b": f(ln2_b),
        "router_group": f(router_group), "router_expert": f(router_expert),
        "expert_w_gate": f(expert_w_gate), "expert_w_up": f(expert_w_up),
        "expert_w_down": f(expert_w_down),
        "attn_bias": bias, "c_ident": ident, "c_ustrict": ustrict, "c_causal": causal,
        "c_ecoff": ecoff,
    }
    in_maps = []
    per_seq = SEQ // (OWN * 128)
    for core in range(NCORES):
        b, q = core // per_seq, core % per_seq
        start = q * OWN * 128
        xin = np.zeros((TOK, D), np.float32)
        if q == 0:
            xin[HALO * 128:] = x[b, 0:OWN * 128]
        else:
            xin[:] = x[b, start - HALO * 128:start + OWN * 128]
        flags = np.zeros((128, 4), np.float32)
        flags[:, 2] = XS_ROWS + np.arange(128)
        flags[:, 3] = 1.0 if q == 0 else 0.0
        flags[:, 0] = 0.0 if q == 0 else 1.0
        flags[:, 1] = NEG if q == 0 else 0.0
        m = dict(shared)
        m["x_in"] = xin
        m["c_flags"] = flags
        in_maps.append(m)
    if _prep_only:
        return in_maps
    if _nlayers not in _CACHE:
        _CACHE[_nlayers] = build(_nlayers)
    nc = _CACHE[_nlayers]
    res = run_bass_kernel_spmd(nc, in_maps, core_ids=list(range(NCORES)))
    outs = [np.asarray(r["out"]) for r in res.results]
    y = np.stack(outs, 0).reshape(2, SEQ, D).astype(np.float32)
    return y
```

```python
import numpy as np
from contextlib import ExitStack
import concourse.bass as bass
import concourse.mybir as mybir
from concourse.bass_utils import run_bass_kernel_spmd

F32 = mybir.dt.float32
BF16 = mybir.dt.bfloat16
I32 = mybir.dt.int32
AF = mybir.ActivationFunctionType
ALU = mybir.AluOpType
AX = mybir.AxisListType

NCORES = 8
D = 1024
SEQ = 16384
DEPTH = 4
HALO = 3
OWN = 32
NCH = HALO + OWN
TOK = NCH * 128
CAP = 384
RB = CAP // 128
NEXP = 32
XS_ROWS = NEXP * CAP
ALPHA = float((2 * DEPTH) ** 0.25)
EPS = 1e-5
NEG = -30000.0
SAME_ENGINE_SYNC = True


class Res:
    __slots__ = ("name", "w", "r", "psum", "small")

    def __init__(self, name, psum=False, small=False):
        self.name = name
        self.psum = psum
        self.small = small
        self.w = None
        self.r = {}


class DSem:
    def __init__(self, sem):
        self.sem = sem
        self.total = 0


class Eng:
    def __init__(self, name, e, sem):
        self.name = name
        self.e = e
        self.sem = sem
        self.cnt = 0
        self.seen = {}

    def wait(self, tok, small=True):
        sem, val = tok
        if sem is self.sem and (self.name == "pe" or not SAME_ENGINE_SYNC or not small):
            return
        k = id(sem)
        if self.seen.get(k, 0) >= val:
            return
        self.e.wait_ge(sem, val)
        self.seen[k] = val


def _deps(reads, writes):
    toks = []
    for r in reads:
        if r.w is not None:
            toks.append((r.w, r.small))
    for w in writes:
        if w.w is not None:
            toks.append((w.w, w.small))
        toks.extend((t, w.small) for t in w.r.values())
    return toks


def _commit(tok, reads, writes):
    for r in reads:
        r.r[id(tok[0])] = tok
    for w in writes:
        w.w = tok
        w.r = {}


def op(E, reads, writes, fn):
    writes = list(writes) + [r for r in reads if r.psum and r not in writes]
    for t, small in _deps(reads, writes):
        E.wait(t, small)
    inst = fn()
    E.cnt += 1
    inst.then_inc(E.sem, 1)
    _commit((E.sem, E.cnt), reads, writes)


def dma(Q, ds, reads, writes, fn, extra=()):
    for t, _sm in list(_deps(reads, writes)) + [(x, True) for x in extra]:
        Q.wait(t)
    inst = fn()
    ds.total += 16
    inst.then_inc(ds.sem, 16)
    tok = (ds.sem, ds.total)
    _commit(tok, reads, writes)
    return tok


def build(nlayers=DEPTH, dbg=False):
    nc = bass.Bass("TRN2", target_bir_lowering=False)
    es = ExitStack()

    def dram_in(name, shape, dt=F32):
        return nc.dram_tensor(name, list(shape), dt, kind="ExternalInput").ap()

    x_in = dram_in("x_in", [TOK, D])
    mix_w_in = dram_in("mix_w_in", [2, D, 2560])
    mix_w_out = dram_in("mix_w_out", [2, D, D])
    gmlp_ln_g = dram_in("gmlp_ln_g", [2, 512])
    gmlp_ln_b = dram_in("gmlp_ln_b", [2, 512])
    gmlp_w_sp = dram_in("gmlp_w_spatial", [2, 4, 128, 128])
    gmlp_b_sp = dram_in("gmlp_b_spatial", [2, 4, 128])
    conv_w = dram_in("conv_w", [2, 3, 512])
    attn_w_qkv = dram_in("attn_w_qkv", [2, D, 1280])
    attn_b_qkv = dram_in("attn_b_qkv", [2, 1280])
    attn_sinks = dram_in("attn_sinks", [2, 16])
    attn_w_o = dram_in("attn_w_o", [2, D, D])
    attn_b_o = dram_in("attn_b_o", [2, D])
    ln1_g = dram_in("ln1_g", [DEPTH, D])
    ln1_b = dram_in("ln1_b", [DEPTH, D])
    ln2_g = dram_in("ln2_g", [DEPTH, D])
    ln2_b = dram_in("ln2_b", [DEPTH, D])
    router_group = dram_in("router_group", [DEPTH, D, 4])
    router_expert = dram_in("router_expert", [DEPTH, D, 32])
    w_gate = dram_in("expert_w_gate", [DEPTH, NEXP, D, 512])
    w_up = dram_in("expert_w_up", [DEPTH, NEXP, D, 512])
    w_down = dram_in("expert_w_down", [DEPTH, NEXP, 512, D])
    attn_bias = dram_in("attn_bias", [128, 16, 256])
    c_ident = dram_in("c_ident", [128, 128])
    c_ustrict = dram_in("c_ustrict", [128, 128])
    c_causal = dram_in("c_causal", [128, 128])
    c_ecoff = dram_in("c_ecoff", [128, 32])
    c_flags = dram_in("c_flags", [128, 4])

    out = nc.dram_tensor("out", [OWN * 128, D], F32, kind="ExternalOutput").ap()
    dbg_out = nc.dram_tensor("dbg", [DEPTH, 1024, D], F32, kind="ExternalOutput").ap() if dbg else None
    XS = nc.dram_tensor("xs_scr", [XS_ROWS + 128, D], BF16, kind="Internal").ap()
    YS = nc.dram_tensor("ys_scr", [XS_ROWS + 128, D], F32, kind="Internal").ap()
    X1 = nc.dram_tensor("x1_scr", [TOK, D], F32, kind="Internal").ap()
    X2 = nc.dram_tensor("x2_scr", [TOK, D], F32, kind="Internal").ap()

    def sb(name, shape, dt=F32):
        t = es.enter_context(nc.sbuf_tensor(name, list(shape), dt))
        return t, Res(name, small=(int(np.prod(shape[1:])) <= 256))

    def newsem(name):
        return es.enter_context(nc.semaphore(name))

    PE = Eng("pe", nc.tensor, newsem("s_pe"))
    ACT = Eng("act", nc.scalar, newsem("s_act"))
    DVE = Eng("dve", nc.vector, newsem("s_dve"))
    POOL = Eng("pool", nc.gpsimd, newsem("s_pool"))
    SP = Eng("sp", nc.sync, newsem("s_sp"))
    _dsn = [0]
    all_ds = []

    def dsem():
        _dsn[0] += 1
        d = DSem(newsem(f"d{_dsn[0]}"))
        all_ds.append(d)
        return d

    def regroup(ds, rlist):
        for r in rlist:
            r.w = (ds.sem, ds.total)

    def barrier():
        engs = [PE, ACT, DVE, POOL, SP]
        for E in engs:
            for Fe in engs:
                if Fe is not E and Fe.cnt > 0:
                    E.wait((Fe.sem, Fe.cnt))
            for d in all_ds:
                if d.total > 0:
                    E.wait((d.sem, d.total))

    banks = []
    for i in range(8):
        t = es.enter_context(nc.psum_tensor(f"bank{i}", [128, 512], F32))
        banks.append((t, Res(f"bank{i}", psum=True)))

    ident_f, r_ident_f = sb("ident_f", [128, 128])
    ident_b, r_ident_b = sb("ident_b", [128, 128], BF16)
    ustr_f, r_ustr_f = sb("ustr_f", [128, 128])
    ustr_b, r_ustr_b = sb("ustr_b", [128, 128], BF16)
    ones_b, r_ones_b = sb("ones_b", [128, 128], BF16)
    causal_f, r_causal = sb("causal_f", [128, 128])
    ecoff, r_ecoff = sb("ecoff", [128, 32])
    flags, r_flags = sb("flags", [128, 4])
    biasB, r_biasB = sb("biasB", [128, 16, 256])
    neghalf, r_neghalf = sb("neghalf", [128, 1])
    cds = dsem()
    dma(SP, cds, [], [r_ident_f], lambda: nc.sync.dma_start(out=ident_f[:], in_=c_ident))
    dma(SP, cds, [], [r_ustr_f], lambda: nc.sync.dma_start(out=ustr_f[:], in_=c_ustrict))
    dma(SP, cds, [], [r_causal], lambda: nc.sync.dma_start(out=causal_f[:], in_=c_causal))
    dma(SP, cds, [], [r_ecoff], lambda: nc.sync.dma_start(out=ecoff[:], in_=c_ecoff))
    dma(SP, cds, [], [r_flags], lambda: nc.sync.dma_start(out=flags[:], in_=c_flags))
    dma(SP, cds, [], [r_biasB], lambda: nc.sync.dma_start(out=biasB[:], in_=attn_bias))
    regroup(cds, [r_ident_f, r_ustr_f, r_causal, r_ecoff, r_flags, r_biasB])
    op(DVE, [r_ident_f], [r_ident_b], lambda: nc.vector.tensor_copy(out=ident_b[:], in_=ident_f[:]))
    op(DVE, [r_ustr_f], [r_ustr_b], lambda: nc.vector.tensor_copy(out=ustr_b[:], in_=ustr_f[:]))
    op(DVE, [], [r_ones_b], lambda: nc.vector.memset(ones_b[:], 1.0))
    op(DVE, [], [r_neghalf], lambda: nc.vector.memset(neghalf[:], -0.5))

    RI, r_RI = sb("RI", [128, NCH, 2])
    DI, r_DI = sb("DI", [128, NCH, 2], I32)
    carryB, r_carry = sb("carryB", [128, 32])

    lnG, r_lnG = sb("lnG", [128, D])
    lnB, r_lnB = sb("lnB", [128, D])
    Wr, r_Wr = sb("Wr", [128, 8, 36])
    lds = dsem()

    RBYTES = 57344
    Rt, _ = sb("Rreg", [128, RBYTES // 4])

    def rview(off, shape, dt):
        nb = int(np.prod(shape)) * (2 if dt == BF16 else 4)
        ap = Rt[:, off // 4:(off + nb) // 4]
        if dt != F32:
            ap = ap.bitcast(dt)
        if len(shape) == 2:
            return ap.rearrange("p (a b) -> p a b", a=shape[0])
        return ap

    EOt, _ = sb("EOreg", [128, 24576 // 4])

    def aview(off, shape, dt, name, small=False):
        nb = int(np.prod(shape)) * (2 if dt == BF16 else 4)
        ap = EOt[:, off // 4:(off + nb) // 4]
        if dt != F32:
            ap = ap.bitcast(dt)
        if len(shape) == 2:
            ap = ap.rearrange("p (a b) -> p a b", a=shape[0])
        elif len(shape) == 3:
            ap = ap.rearrange("p (a b c) -> p a b c", a=shape[0], b=shape[1])
        return ap, Res(name, small=small)

    Win, r_Win = rview(0, [8, 2560], BF16), Res("Win")
    Wout, r_Wout = rview(40960, [8, D], BF16), Res("Wout")
    gBg, r_gBg = aview(12352, [512], F32, "gBg")
    gBb, r_gBb = aview(14400, [512], F32, "gBb")
    bspB, r_bspB = aview(16448, [4, 128], F32, "bspB")
    wsT, r_wsT = aview(18496, [4, 128], BF16, "wsT")
    wsp_raw, r_wsp_raw = aview(19520, [4, 128], F32, "wsp_raw")
    convw, r_convw = sb("convw", [128, 4, 3])
    qb, r_qb = sb("qb", [128, 8])
    kb2, r_kb2 = sb("kb2", [128, 2])
    vbB, r_vbB = aview(19968, [128], F32, "vbB", small=True)
    boB, r_boB = aview(20480, [D], F32, "boB")
    sinkB, r_sinkB = sb("sinkB", [128, 16])
    wds = dsem()

    Wg = [sb(f"Wg{i}", [128, 8, 512], BF16) for i in range(2)]
    Wu = [sb(f"Wu{i}", [128, 8, 512], BF16) for i in range(2)]
    Wd = [sb(f"Wd{i}", [128, 4, D], BF16) for i in range(2)]
    ewds = [dsem(), dsem()]
    ewds2 = [dsem(), dsem()]

    xt = [sb(f"xt{i}", [128, D]) for i in range(2)]
    xtds = [dsem(), dsem()]
    xT, r_xT = sb("xT", [128, 8, 128], BF16)
    uT, r_uT = aview(0, [4, 128], BF16, "uT")
    vv, r_vv = aview(1024, [512], F32, "vv")
    vn, r_vn = aview(3072, [512], BF16, "vn")
    hb, r_hb = aview(4096, [512], F32, "hb")
    zz = [aview(6144 + i * 2080, [4, 130], F32, f"zz{i}", small=True) for i in range(2)]
    acc, r_acc = aview(10304, [4, 128], F32, "acc", small=True)

    yT, r_yT = sb("yT", [128, 8, 128], BF16)
    tt, r_tt = sb("tt", [128, D])
    x1, r_x1 = tt, r_tt
    x1T, r_x1T = sb("x1T", [128, 8, 128])
    x1b, r_x1b = sb("x1b", [128, D], BF16)
    st6_, mv_, rstd_, nmr_ = ([sb(f"{n}{i}", sh) for i in range(2)] for n, sh in (("st6", [128, 12]), ("mv", [128, 2]), ("rstd", [128, 1]), ("nmr", [128, 1])))
    st6, r_st6 = st6_[0]
    mv, r_mv = mv_[0]
    rstd, r_rstd = rstd_[0]
    nmr, r_nmr = nmr_[0]
    lg, r_lg = sb("lg", [128, 36])
    rs, r_rs = sb("rs", [128, 64])
    selm, r_selm = sb("selm", [128, 32])
    top8, r_top8 = sb("top8", [128, 8])
    oh1, r_oh1 = sb("oh1", [128, 32])
    oh2, r_oh2 = sb("oh2", [128, 32])
    Aoh, r_Aoh = sb("Aoh", [128, 32], BF16)
    posf, r_posf = sb("posf", [128, 32])
    tmp32, r_tmp32 = sb("tmp32", [128, 32])
    oh1_s, r_ohs = sb("oh1_s", [128, 32])
    destf, r_destf = sb("destf", [128, 2])
    qT, r_qT = aview(0, [8, 128], BF16, "qT")
    kT, r_kT = aview(2048, [2, 2, 128], BF16, "kT")
    r_kTs = [Res("kTs0"), Res("kTs1")]
    vtok, r_vtok_ = aview(3072, [2, 128], BF16, "vtok")
    r_vts = [Res("vts0"), Res("vts1")]
    ssb_ = [aview(3584 + i * 4096, [4, 256], F32, f"ssb{i}") for i in range(2)]
    pp_ = [aview(11776 + i * 2048, [4, 256], BF16, f"pp{i}") for i in range(2)]
    r_mrow_ = [Res(f"mrow{i}", small=True) for i in range(4)]
    r_nmrow_ = [Res(f"nmrow{i}", small=True) for i in range(4)]
    r_rsum_ = [Res(f"rsum{i}", small=True) for i in range(4)]
    r_esink_ = [Res(f"esink{i}", small=True) for i in range(4)]
    r_rden_ = [Res(f"rden{i}", small=True) for i in range(4)]
    pT, r_pT = aview(15872, [8, 128], BF16, "pT")
    osb, r_osb = aview(17920, [D], BF16, "osb")
    mrow, r_mrow = sb("mrow", [128, 16])
    nmrow, r_nmrow = sb("nmrow", [128, 16])
    rsum, r_rsum = sb("rsum", [128, 16])
    esink, r_esink = sb("esink", [128, 16])
    rden, r_rden = sb("rden", [128, 16])
    y1 = [(rview(i * 4096, [D], F32), Res(f"y1_{i}")) for i in range(2)]
    y2 = [(rview(8192 + i * 4096, [D], F32), Res(f"y2_{i}")) for i in range(2)]
    gds = [dsem(), dsem()]
    xc = [(rview(16384 + i * 4096, [D], F32), Res(f"xc{i}")) for i in range(2)]
    xcds = [dsem(), dsem()]
    x2t_ = [(rview(24576 + i * 4096, [D], F32), Res(f"x2t{i}")) for i in range(2)]
    ttc_ = [(rview(32768 + i * 4096, [D], F32), Res(f"ttc{i}")) for i in range(2)]
    xs = [(rview(i * 6144, [RB, D], BF16), Res(f"xs{i}")) for i in range(2)]
    xsds = [dsem(), dsem()]
    xsT, r_xsT = rview(12288, [8, CAP], BF16), Res("xsT")
    sg = [(rview(18432 + i * 1536, [CAP], F32), Res(f"sg{i}")) for i in range(2)]
    hT, r_hT = rview(21504, [4, CAP], BF16), Res("hT")
    ys = [(rview(24576 + i * 12288, [RB, D], F32), Res(f"ys{i}")) for i in range(2)]
    ysds = [dsem(), dsem()]
    sc_ds = dsem()
    st_ds = dsem()
    r_X1 = [Res(f"X1_{c}") for c in range(NCH)]
    r_X2 = [Res(f"X2_{c}") for c in range(NCH)]
    out_toks = []

    V = nc.vector
    S = nc.scalar
    G = nc.gpsimd
    T = nc.tensor

    def layer_norm(src, r_src, dst, r_dst, gB, r_gB, bB, r_bB, par=0):
        (st6, r_st6), (mv, r_mv), (rstd, r_rstd), (nmr, r_nmr) = st6_[par], mv_[par], rstd_[par], nmr_[par]
        for h in range(2):
            op(DVE, [r_src], [r_st6], lambda h=h: V.bn_stats(out=st6[:, h * 6:(h + 1) * 6], in_=src[:, h * 512:(h + 1) * 512]))
        op(DVE, [r_st6], [r_mv], lambda: V.bn_aggr(out=mv[:], in_=st6[:]))
        op(DVE, [r_mv], [r_rstd], lambda: V.tensor_scalar(out=rstd[:], in0=mv[:, 1:2], scalar1=EPS, scalar2=None, op0=ALU.add))
        op(POOL, [r_rstd, r_neghalf], [r_rstd], lambda: G.tensor_tensor(out=rstd[:], in0=rstd[:], in1=neghalf[:], op=ALU.pow))
        op(DVE, [r_mv, r_rstd], [r_nmr], lambda: V.scalar_tensor_tensor(out=nmr[:], in0=mv[:, 0:1], scalar=-1.0, in1=rstd[:], op0=ALU.mult, op1=ALU.mult))
        op(ACT, [r_src, r_rstd, r_nmr], [r_dst], lambda: S.activation(out=dst[:], in_=src[:], func=AF.Identity, bias=nmr[:], scale=rstd[:]))
        op(DVE, [r_dst, r_gB], [r_dst], lambda: V.tensor_tensor(out=dst[:], in0=dst[:], in1=gB[:], op=ALU.mult))
        op(DVE, [r_dst, r_bB], [r_dst], lambda: V.tensor_tensor(out=dst[:], in0=dst[:], in1=bB[:], op=ALU.add))

    def bcast_load(dst, r_dst, src_vec):
        dma(SP, lds, [], [r_dst], lambda: nc.sync.dma_start(out=dst, in_=src_vec.partition_broadcast(128)))

    def load_expert(l, e, part=None):
        s = e % 2
        (wg, r_wg), (wu, r_wu), (wd, r_wd) = Wg[s], Wu[s], Wd[s]
        if part in (None, 0):
            dma(POOL, ewds[s], [], [r_wg], lambda: G.dma_start(out=wg[:], in_=w_gate[l, e].rearrange("(kt p) n -> p kt n", p=128)))
            dma(POOL, ewds[s], [], [r_wu], lambda: G.dma_start(out=wu[:], in_=w_up[l, e].rearrange("(kt p) n -> p kt n", p=128)))
            regroup(ewds[s], [r_wg, r_wu])
        if part in (None, 1):
            dma(POOL, ewds2[s], [], [r_wd], lambda: G.dma_start(out=wd[:], in_=w_down[l, e].rearrange("(kt p) n -> p kt n", p=128)))

    def router_and_scatter(l, c):
        b0, r_b0 = banks[0]
        b1, r_b1 = banks[1]
        b4, r_b4 = banks[4]
        for kt in range(8):
            bt, r_bt = (b0, r_b0) if kt < 4 else (b1, r_b1)
            op(PE, [r_x1, r_ident_f], [r_bt], lambda kt=kt, bt=bt: T.transpose(out=bt[:, (kt % 4) * 128:(kt % 4 + 1) * 128], in_=x1[:, kt * 128:(kt + 1) * 128], identity=ident_f[:]))
        op(ACT, [r_b0], [r_x1T], lambda: S.copy(out=x1T[:, 0:4, :], in_=b0[:].rearrange("p (k t) -> p k t", k=4)))
        op(ACT, [r_b1], [r_x1T], lambda: S.copy(out=x1T[:, 4:8, :], in_=b1[:].rearrange("p (k t) -> p k t", k=4)))
        op(ACT, [r_x1], [r_x1b], lambda: S.copy(out=x1b[:], in_=x1[:]))
        dma(SP, st_ds, [r_x1], [r_X1[c]], lambda: nc.sync.dma_start(out=X1[c * 128:(c + 1) * 128, :], in_=x1[:]))
        for kt in range(8):
            op(PE, [r_x1T, r_Wr], [r_b4], lambda kt=kt: T.matmul(b4[:, 0:36], lhsT=x1T[:, kt, :], rhs=Wr[:, kt, :], start=(kt == 0), stop=(kt == 7)))
        op(DVE, [r_b4], [r_lg], lambda: V.tensor_copy(out=lg[:], in_=b4[:, 0:36]))
        gmax, ngmax, gsum, gp = rs[:, 0:1], rs[:, 1:2], rs[:, 2:3], rs[:, 3:4]
        ohg, negm, gex = rs[:, 4:8], rs[:, 8:12], rs[:, 12:16]
        op(DVE, [r_lg], [r_rs], lambda: V.tensor_reduce(out=gmax, in_=lg[:, 0:4], axis=AX.X, op=ALU.max))
        op(DVE, [r_rs], [r_rs], lambda: V.tensor_scalar(out=ngmax, in0=gmax, scalar1=-1.0, scalar2=None, op0=ALU.mult))
        op(ACT, [r_lg, r_rs], [r_rs], lambda: S.activation(out=gex, in_=lg[:, 0:4], func=AF.Exp, bias=ngmax, scale=1.0, accum_out=gsum))
        op(DVE, [r_rs], [r_rs], lambda: V.reciprocal(out=gp, in_=gsum))
        op(DVE, [r_lg, r_rs], [r_rs], lambda: V.tensor_scalar(out=ohg, in0=lg[:, 0:4], scalar1=gmax, scalar2=None, op0=ALU.is_equal))
        op(DVE, [r_rs], [r_rs], lambda: V.tensor_scalar(out=negm, in0=ohg, scalar1=-1.0, scalar2=-NEG, op0=ALU.add, op1=ALU.mult))
        op(DVE, [r_lg, r_rs], [r_selm], lambda: V.tensor_tensor(out=selm[:].rearrange("p (g e) -> p g e", g=4), in0=lg[:, 4:36].rearrange("p (g e) -> p g e", g=4), in1=negm.unsqueeze(2).to_broadcast([128, 4, 8]), op=ALU.add))
        op(DVE, [r_selm], [r_top8], lambda: V.max(out=top8[:], in_=selm[:]))
        op(DVE, [r_selm, r_top8], [r_oh1], lambda: V.tensor_scalar(out=oh1[:], in0=selm[:], scalar1=top8[:, 0:1], scalar2=None, op0=ALU.is_equal))
        op(DVE, [r_selm, r_top8], [r_oh2], lambda: V.tensor_scalar(out=oh2[:], in0=selm[:], scalar1=top8[:, 1:2], scalar2=None, op0=ALU.is_equal))
        op(DVE, [r_oh1, r_oh2], [r_Aoh], lambda: V.tensor_tensor(out=Aoh[:], in0=oh1[:], in1=oh2[:], op=ALU.add))
        if c < HALO:
            op(DVE, [r_Aoh, r_flags], [r_Aoh], lambda: V.tensor_scalar(out=Aoh[:], in0=Aoh[:], scalar1=flags[:, 0:1], scalar2=None, op0=ALU.mult))
        dlt, ex, w1 = rs[:, 16:17], rs[:, 17:18], rs[:, 18:19]
        op(DVE, [r_top8], [r_rs], lambda: V.tensor_tensor(out=dlt, in0=top8[:, 1:2], in1=top8[:, 0:1], op=ALU.subtract))
        op(ACT, [r_rs], [r_rs], lambda: S.activation(out=ex, in_=dlt, func=AF.Exp))
        op(DVE, [r_rs], [r_rs], lambda: V.tensor_scalar(out=ex, in0=ex, scalar1=1.0, scalar2=None, op0=ALU.add))
        op(DVE, [r_rs], [r_rs], lambda: V.reciprocal(out=w1, in_=ex))
        op(DVE, [r_rs], [r_RI], lambda: V.tensor_tensor(out=RI[:, c, 0:1], in0=w1, in1=gp, op=ALU.mult))
        op(DVE, [r_rs, r_RI], [r_RI], lambda: V.tensor_tensor(out=RI[:, c, 1:2], in0=gp, in1=RI[:, c, 0:1], op=ALU.subtract))
        op(PE, [r_ustr_b, r_Aoh], [r_b4], lambda: T.matmul(b4[:, 64:96], lhsT=ustr_b[:], rhs=Aoh[:], start=True, stop=True))
        op(PE, [r_ones_b, r_Aoh], [r_b4], lambda: T.matmul(b4[:, 128:160], lhsT=ones_b[:], rhs=Aoh[:], start=True, stop=True))
        op(DVE, [r_b4, r_carry], [r_posf], lambda: V.tensor_tensor(out=posf[:], in0=b4[:, 64:96], in1=carryB[:], op=ALU.add))
        op(DVE, [r_b4, r_carry], [r_carry], lambda: V.tensor_tensor(out=carryB[:], in0=b4[:, 128:160], in1=carryB[:], op=ALU.add))
        op(DVE, [r_posf], [r_tmp32], lambda: V.tensor_scalar(out=tmp32[:], in0=posf[:], scalar1=float(CAP) - 0.5, scalar2=None, op0=ALU.is_gt))
        if c < HALO:
            op(DVE, [r_tmp32, r_flags], [r_tmp32], lambda: V.tensor_scalar(out=tmp32[:], in0=tmp32[:], scalar1=flags[:, 3:4], scalar2=None, op0=ALU.max))
        op(DVE, [r_posf, r_ecoff], [r_posf], lambda: V.tensor_tensor(out=posf[:], in0=posf[:], in1=ecoff[:], op=ALU.add))
        op(DVE, [r_posf, r_flags], [r_ohs], lambda: V.tensor_scalar(out=oh1_s[:], in0=posf[:], scalar1=flags[:, 2:3], scalar2=None, op0=ALU.subtract))
        op(DVE, [r_ohs, r_tmp32], [r_tmp32], lambda: V.tensor_tensor(out=tmp32[:], in0=oh1_s[:], in1=tmp32[:], op=ALU.mult))
        op(DVE, [r_posf, r_tmp32], [r_posf], lambda: V.tensor_tensor(out=posf[:], in0=posf[:], in1=tmp32[:], op=ALU.subtract))
        op(DVE, [r_posf, r_oh1], [r_tmp32], lambda: V.tensor_tensor(out=tmp32[:], in0=posf[:], in1=oh1[:], op=ALU.mult))
        op(DVE, [r_tmp32], [r_destf], lambda: V.tensor_reduce(out=destf[:, 0:1], in_=tmp32[:], axis=AX.X, op=ALU.add))
        op(DVE, [r_posf, r_oh2], [r_tmp32], lambda: V.tensor_tensor(out=tmp32[:], in0=posf[:], in1=oh2[:], op=ALU.mult))
        op(DVE, [r_tmp32], [r_destf], lambda: V.tensor_reduce(out=destf[:, 1:2], in_=tmp32[:], axis=AX.X, op=ALU.add))
        op(DVE, [r_destf], [r_DI], lambda: V.tensor_copy(out=DI[:, c, :], in_=destf[:]))
        for k in range(2):
            dma(POOL, sc_ds, [r_x1b, r_DI], [], lambda k=k: G.indirect_dma_start(
                out=XS, out_offset=bass.IndirectOffsetOnAxis(ap=DI[:, c, k:k + 1], axis=0),
                in_=x1b[:], in_offset=None))

    def transposes_x(src, r_src):
        b0, r_b0 = banks[0]
        b1, r_b1 = banks[1]
        for kt in range(8):
            bt, r_bt = (b0, r_b0) if kt < 4 else (b1, r_b1)
            op(PE, [r_src, r_ident_f], [r_bt], lambda kt=kt, bt=bt: T.transpose(out=bt[:, (kt % 4) * 128:(kt % 4 + 1) * 128], in_=src[:, kt * 128:(kt + 1) * 128], identity=ident_f[:]))
        op(ACT, [r_b0], [r_xT], lambda: S.copy(out=xT[:, 0:4, :], in_=b0[:].rearrange("p (k t) -> p k t", k=4)))
        op(ACT, [r_b1], [r_xT], lambda: S.copy(out=xT[:, 4:8, :], in_=b1[:].rearrange("p (k t) -> p k t", k=4)))

    def mixer_even(l, c, src, r_src, part):
        b2, r_b2 = banks[2]
        b3, r_b3 = banks[3]
        b4, r_b4 = banks[4]
        if part == 0:
            for j in range(4):
                for kt in range(8):
                    op(PE, [r_Win, r_xT], [r_b2], lambda j=j, kt=kt: T.matmul(b2[:, j * 128:(j + 1) * 128], lhsT=Win[:, kt, j * 128:(j + 1) * 128], rhs=xT[:, kt, :], start=(kt == 0), stop=(kt == 7)))
            for kt in range(8):
                op(PE, [r_Win, r_xT], [r_b3], lambda kt=kt: T.matmul(b3[:, :], lhsT=xT[:, kt, :], rhs=Win[:, kt, 512:1024], start=(kt == 0), stop=(kt == 7)))
            for j in range(12):
                bt, r_bt = banks[5 + j // 4]
                for kt in range(8):
                    op(PE, [r_Win, r_xT], [r_bt], lambda j=j, kt=kt, bt=bt: T.matmul(bt[:, (j % 4) * 128:(j % 4 + 1) * 128], lhsT=Win[:, kt, 1024 + j * 128:1024 + (j + 1) * 128], rhs=xT[:, kt, :], start=(kt == 0), stop=(kt == 7)))
            op(ACT, [r_b2], [r_uT], lambda: S.activation(out=uT[:].rearrange("p g t -> p (g t)"), in_=b2[:], func=AF.Gelu))
            op(ACT, [r_b3], [r_vv], lambda: S.activation(out=vv[:], in_=b3[:], func=AF.Gelu))
            b7, r_b7 = banks[7]
            op(ACT, [r_b7], [r_hb], lambda: S.copy(out=hb[:], in_=b7[:]))
            return
        op(DVE, [r_vv], [r_st6], lambda: V.bn_stats(out=st6[:, 0:6], in_=vv[:]))
        op(DVE, [r_st6], [r_mv], lambda: V.bn_aggr(out=mv[:], in_=st6[:, 0:6]))
        op(DVE, [r_mv], [r_rstd], lambda: V.tensor_scalar(out=rstd[:], in0=mv[:, 1:2], scalar1=EPS, scalar2=None, op0=ALU.add))
        op(POOL, [r_rstd, r_neghalf], [r_rstd], lambda: G.tensor_tensor(out=rstd[:], in0=rstd[:], in1=neghalf[:], op=ALU.pow))
        op(DVE, [r_vv, r_mv, r_rstd], [r_vv], lambda: V.tensor_scalar(out=vv[:], in0=vv[:], scalar1=mv[:, 0:1], scalar2=rstd[:], op0=ALU.subtract, op1=ALU.mult))
        op(DVE, [r_vv, r_gBg], [r_vv], lambda: V.tensor_tensor(out=vv[:], in0=vv[:], in1=gBg[:], op=ALU.mult))
        op(DVE, [r_vv, r_gBb], [r_vn], lambda: V.tensor_tensor(out=vn[:], in0=vv[:], in1=gBb[:], op=ALU.add))
        for g in range(4):
            op(PE, [r_vn, r_wsT], [r_b4], lambda g=g: T.matmul(b4[:, g * 128:(g + 1) * 128], lhsT=vn[:, g * 128:(g + 1) * 128], rhs=wsT[:, g, :], start=True, stop=True))
        op(DVE, [r_b4, r_bspB], [r_acc], lambda: V.tensor_tensor(out=acc[:].rearrange("p g t -> p (g t)"), in0=b4[:], in1=bspB[:].rearrange("p g t -> p (g t)"), op=ALU.add))
        op(DVE, [r_acc, r_uT], [r_yT], lambda: V.tensor_tensor(out=yT[:, 0:4, :], in0=acc[:], in1=uT[:], op=ALU.mult))
        b5, r_b5 = banks[5]
        b6, r_b6 = banks[6]
        b7, r_b7 = banks[7]
        (zc, r_zc), (zp, r_zp) = zz[c % 2], zz[(c + 1) % 2]
        op(DVE, [r_b6, r_hb], [r_zc], lambda: V.tensor_tensor(out=zc[:, :, 2:130], in0=b6[:].rearrange("p (g t) -> p g t", g=4), in1=hb[:].rearrange("p (g t) -> p g t", g=4), op=ALU.mult))
        if c == 0:
            op(DVE, [], [r_zc], lambda: V.memset(zc[:, :, 0:2], 0.0))
        elif c == HALO:
            op(DVE, [r_zp, r_flags], [r_zc], lambda: V.tensor_scalar(out=zc[:, :, 0:2], in0=zp[:, :, 128:130], scalar1=flags[:, 0:1], scalar2=None, op0=ALU.mult))
        else:
            op(DVE, [r_zp], [r_zc], lambda: V.tensor_copy(out=zc[:, :, 0:2], in_=zp[:, :, 128:130]))
        for g in range(4):
            op(DVE, [r_zc, r_convw], [r_acc], lambda g=g: V.tensor_scalar(out=acc[:, g, :], in0=zc[:, g, 0:128], scalar1=convw[:, g, 0:1], scalar2=None, op0=ALU.mult))
            op(DVE, [r_zc, r_convw, r_acc], [r_acc], lambda g=g: V.scalar_tensor_tensor(out=acc[:, g, :], in0=zc[:, g, 1:129], scalar=convw[:, g, 1:2], in1=acc[:, g, :], op0=ALU.mult, op1=ALU.add))
            op(DVE, [r_zc, r_convw, r_acc], [r_acc], lambda g=g: V.scalar_tensor_tensor(out=acc[:, g, :], in0=zc[:, g, 2:130], scalar=convw[:, g, 2:3], in1=acc[:, g, :], op0=ALU.mult, op1=ALU.add))
        op(DVE, [r_b5, r_acc], [r_yT], lambda: V.tensor_tensor(out=yT[:, 4:8, :], in0=b5[:].rearrange("p (g t) -> p g t", g=4), in1=acc[:], op=ALU.mult))
        for h in range(2):
            bt, r_bt = banks[2 + h]
            for kt in range(8):
                op(PE, [r_yT, r_Wout], [r_bt], lambda h=h, kt=kt, bt=bt: T.matmul(bt[:, :], lhsT=yT[:, kt, :], rhs=Wout[:, kt, h * 512:(h + 1) * 512], start=(kt == 0), stop=(kt == 7)))
        for h in range(2):
            bt, r_bt = banks[2 + h]
            op(DVE, [r_src, r_bt], [r_tt], lambda h=h, bt=bt: V.scalar_tensor_tensor(out=tt[:, h * 512:(h + 1) * 512], in0=src[:, h * 512:(h + 1) * 512], scalar=ALPHA, in1=bt[:, :], op0=ALU.mult, op1=ALU.add))

    def mixer_odd(l, c, src, r_src, part):
        b2, r_b2 = banks[2]
        b3, r_b3 = banks[3]
        b4, r_b4 = banks[4]
        sl, ps = c % 2, (c + 1) % 2
        if part == 0:
            for j in range(8):
                bt, r_bt = banks[2 + j // 4]
                for kt in range(8):
                    op(PE, [r_Win, r_xT], [r_bt], lambda j=j, kt=kt, bt=bt: T.matmul(bt[:, (j % 4) * 128:(j % 4 + 1) * 128], lhsT=Win[:, kt, j * 128:(j + 1) * 128], rhs=xT[:, kt, :], start=(kt == 0), stop=(kt == 7)))
            for kv in range(2):
                for kt in range(8):
                    op(PE, [r_Win, r_xT], [r_b4], lambda kv=kv, kt=kt: T.matmul(b4[:, kv * 128:(kv + 1) * 128], lhsT=Win[:, kt, 1024 + kv * 128:1024 + (kv + 1) * 128], rhs=xT[:, kt, :], start=(kt == 0), stop=(kt == 7)))
            for kt in range(8):
                op(PE, [r_Win, r_xT], [r_b4], lambda kt=kt: T.matmul(b4[:, 256:384], lhsT=xT[:, kt, :], rhs=Win[:, kt, 1280:1408], start=(kt == 0), stop=(kt == 7)))
            for j in range(8):
                bt, r_bt = banks[2 + j // 4]
                op(ACT, [r_bt, r_qb], [r_qT], lambda j=j, bt=bt: S.activation(out=qT[:, j, :], in_=bt[:, (j % 4) * 128:(j % 4 + 1) * 128], func=AF.Identity, bias=qb[:, j:j + 1], scale=0.125))
            for kv in range(2):
                op(ACT, [r_b4, r_kb2], [r_kTs[sl]], lambda kv=kv: S.activation(out=kT[:, kv, sl, :], in_=b4[:, kv * 128:(kv + 1) * 128], func=AF.Identity, bias=kb2[:, kv:kv + 1], scale=1.0))
            op(DVE, [r_b4, r_vbB], [r_vts[sl]], lambda: V.tensor_tensor(out=vtok[:, sl, :], in0=b4[:, 256:384], in1=vbB[:], op=ALU.add))
            if c == 0:
                for kv in range(2):
                    op(DVE, [], [r_kTs[ps]], lambda kv=kv: V.memset(kT[:, kv, ps, :], 0.0))
                op(DVE, [], [r_vts[ps]], lambda: V.memset(vtok[:, ps, :], 0.0))
            return
        b7, r_b7 = banks[7]
        b7b = b7[:].bitcast(BF16)

        def att_s1(hg):
            sset = hg % 2
            (ssbx, r_ssbx), (ppx, r_ppx) = ssb_[sset], pp_[sset]
            (sb0, r_sb0), (sb1, r_sb1) = (banks[5], banks[6]) if sset == 0 else (banks[2], banks[3])
            for hh in range(4):
                h = hg * 4 + hh
                bt, r_bt = (sb0, r_sb0) if hh % 2 == 0 else (sb1, r_sb1)
                pr = (h % 2) * 64
                kv = h // 8
                for blk, slot in ((0, ps), (1, sl)):
                    op(PE, [r_qT, r_kTs[slot]], [r_bt], lambda h=h, bt=bt, pr=pr, kv=kv, blk=blk, slot=slot, hh=hh: T.matmul(
                        bt[:, (hh // 2) * 256 + blk * 128:(hh // 2) * 256 + (blk + 1) * 128],
                        lhsT=qT[pr:pr + 64, h // 2, :], rhs=kT[pr:pr + 64, kv, slot, :], start=True, stop=True))
            for par in range(2):
                bt, r_bt = (sb0, r_sb0) if par == 0 else (sb1, r_sb1)
                for i2 in range(2):
                    hh = par + 2 * i2
                    op(DVE, [r_bt, r_biasB], [r_ssbx], lambda bt=bt, hh=hh, i2=i2: V.tensor_tensor(out=ssbx[:, hh, :], in0=bt[:, i2 * 256:(i2 + 1) * 256], in1=biasB[:, hg * 4 + hh, :], op=ALU.add))
            if c == HALO:
                op(DVE, [r_ssbx, r_flags], [r_ssbx], lambda: V.tensor_scalar(out=ssbx[:, :, 0:128], in0=ssbx[:, :, 0:128], scalar1=flags[:, 1:2], scalar2=None, op0=ALU.add))
            if c == 0:
                op(DVE, [r_ssbx], [r_ssbx], lambda: V.tensor_scalar(out=ssbx[:, :, 0:128], in0=ssbx[:, :, 0:128], scalar1=NEG, scalar2=None, op0=ALU.add))
            h4 = slice(hg * 4, hg * 4 + 4)
            r_m, r_nm, r_rs_ = r_mrow_[hg], r_nmrow_[hg], r_rsum_[hg]
            op(DVE, [r_ssbx], [r_m], lambda: V.tensor_reduce(out=mrow[:, h4], in_=ssbx[:], axis=AX.X, op=ALU.max))
            op(DVE, [r_m, r_sinkB], [r_m], lambda: V.tensor_tensor(out=mrow[:, h4], in0=mrow[:, h4], in1=sinkB[:, h4], op=ALU.max))
            op(DVE, [r_m], [r_nm], lambda: V.tensor_scalar(out=nmrow[:, h4], in0=mrow[:, h4], scalar1=-1.0, scalar2=None, op0=ALU.mult))
            for hh in range(4):
                h = hg * 4 + hh
                op(ACT, [r_ssbx, r_nm], [r_ppx, r_rs_], lambda h=h, hh=hh: S.activation(out=ppx[:, hh, :], in_=ssbx[:, hh, :], func=AF.Exp, bias=nmrow[:, h:h + 1], scale=1.0, accum_out=rsum[:, h:h + 1]))

        def att_s2(hg):
            sset = hg % 2
            (ppx, r_ppx) = pp_[sset]
            h4 = slice(hg * 4, hg * 4 + 4)
            r_m, r_rs_, r_es, r_rd = r_mrow_[hg], r_rsum_[hg], r_esink_[hg], r_rden_[hg]
            for hh in range(4):
                for blk in range(2):
                    i = hh * 2 + blk
                    op(PE, [r_ppx, r_ident_b], [r_b7], lambda hh=hh, blk=blk, i=i: T.transpose(out=b7b[:, i * 128:(i + 1) * 128], in_=ppx[:, hh, blk * 128:(blk + 1) * 128], identity=ident_b[:]))
            op(ACT, [r_b7], [r_pT], lambda: S.copy(out=pT[:].rearrange("p i q -> p (i q)"), in_=b7b))
            for hh in range(4):
                h = hg * 4 + hh
                kv = h // 8
                for blk, slot in ((0, ps), (1, sl)):
                    op(PE, [r_pT, r_vts[slot]], [r_b4], lambda hh=hh, kv=kv, blk=blk, slot=slot: T.matmul(
                        b4[:, hh * 64:(hh + 1) * 64], lhsT=pT[:, hh * 2 + blk, :], rhs=vtok[:, slot, kv * 64:(kv + 1) * 64], start=(blk == 0), stop=(blk == 1)))
            op(DVE, [r_sinkB, r_m], [r_es], lambda: V.tensor_tensor(out=esink[:, h4], in0=sinkB[:, h4], in1=mrow[:, h4], op=ALU.subtract))
            op(ACT, [r_es], [r_es], lambda: S.activation(out=esink[:, h4], in_=esink[:, h4], func=AF.Exp))
            op(DVE, [r_es, r_rs_], [r_es], lambda: V.tensor_tensor(out=esink[:, h4], in0=esink[:, h4], in1=rsum[:, h4], op=ALU.add))
            op(DVE, [r_es], [r_rd], lambda: V.reciprocal(out=rden[:, h4], in_=esink[:, h4]))
            op(DVE, [r_b4, r_rd], [r_osb], lambda: V.tensor_tensor(out=osb[:, hg * 256:(hg + 1) * 256].rearrange("p (h d) -> p h d", h=4), in0=b4[:, 0:256].rearrange("p (h d) -> p h d", h=4), in1=rden[:, h4].unsqueeze(2).to_broadcast([128, 4, 64]), op=ALU.mult))

        att_s1(0)
        for hg in range(4):
            if hg + 1 < 4:
                att_s1(hg + 1)
            att_s2(hg)
        b7, r_b7 = banks[7]
        b7b = b7[:].bitcast(BF16)
        for kt in range(8):
            op(PE, [r_osb, r_ident_b], [r_b7], lambda kt=kt: T.transpose(out=b7b[:, kt * 128:(kt + 1) * 128], in_=osb[:, kt * 128:(kt + 1) * 128], identity=ident_b[:]))
        op(ACT, [r_b7], [r_yT], lambda: S.copy(out=yT[:].rearrange("p k t -> p (k t)"), in_=b7b))
        for h in range(2):
            bt, r_bt = banks[2 + h]
            for kt in range(8):
                op(PE, [r_yT, r_Wout], [r_bt], lambda h=h, kt=kt, bt=bt: T.matmul(bt[:, :], lhsT=yT[:, kt, :], rhs=Wout[:, kt, h * 512:(h + 1) * 512], start=(kt == 0), stop=(kt == 7)))
        for h in range(2):
            bt, r_bt = banks[2 + h]
            op(DVE, [r_src, r_bt], [r_tt], lambda h=h, bt=bt: V.scalar_tensor_tensor(out=tt[:, h * 512:(h + 1) * 512], in0=src[:, h * 512:(h + 1) * 512], scalar=ALPHA, in1=bt[:, :], op0=ALU.mult, op1=ALU.add))
        op(POOL, [r_tt, r_boB], [r_tt], lambda: G.tensor_tensor(out=tt[:], in0=tt[:], in1=boB[:], op=ALU.add))

    def load_layer_consts(l):
        i = l // 2
        bcast_load(lnG[:], r_lnG, ln1_g[l])
        bcast_load(lnB[:], r_lnB, ln1_b[l])
        dma(SP, lds, [], [r_Wr], lambda: nc.sync.dma_start(out=Wr[:, :, 0:4], in_=router_group[l].rearrange("(kt p) n -> p kt n", p=128)))
        dma(SP, lds, [], [r_Wr], lambda: nc.sync.dma_start(out=Wr[:, :, 4:36], in_=router_expert[l].rearrange("(kt p) n -> p kt n", p=128)))
        op(DVE, [], [r_carry], lambda: V.memset(carryB[:], 0.0))
        if l % 2 == 0:
            for h in range(2):
                dma(POOL, wds, [], [r_Win], lambda h=h: G.dma_start(out=Win[:, :, h * 1280:(h + 1) * 1280], in_=mix_w_in[i][:, h * 1280:(h + 1) * 1280].rearrange("(kt p) n -> p kt n", p=128)))
            dma(POOL, wds, [], [r_Wout], lambda: G.dma_start(out=Wout[:], in_=mix_w_out[i].rearrange("(kt p) n -> p kt n", p=128)))
            bcast_load(gBg[:], r_gBg, gmlp_ln_g[i])
            bcast_load(gBb[:], r_gBb, gmlp_ln_b[i])
            bcast_load(bspB[:].rearrange("p g t -> p (g t)"), r_bspB, gmlp_b_sp[i].rearrange("g t -> (g t)"))
            dma(SP, lds, [], [r_wsp_raw], lambda: nc.sync.dma_start(out=wsp_raw[:], in_=gmlp_w_sp[i].rearrange("g i j -> i g j")))
            for k in range(3):
                dma(SP, lds, [], [r_convw], lambda k=k: nc.sync.dma_start(out=convw[:, :, k:k + 1], in_=conv_w[i, k].rearrange("(g p o) -> p g o", p=128, o=1), allow_slow_non_contiguous=True))
            regroup(lds, [r_lnG, r_lnB, r_Wr, r_gBg, r_gBb, r_bspB, r_wsp_raw, r_convw])
            regroup(wds, [r_Win, r_Wout])
            b4, r_b4 = banks[4]
            for g in range(4):
                op(PE, [r_wsp_raw, r_ident_f], [r_b4], lambda g=g: T.transpose(out=b4[:, g * 128:(g + 1) * 128], in_=wsp_raw[:, g, :], identity=ident_f[:]))
            op(DVE, [r_b4, r_causal], [r_wsT], lambda: V.tensor_tensor(out=wsT[:], in0=b4[:].rearrange("p (g i) -> p g i", g=4), in1=causal_f[:].unsqueeze(1).to_broadcast([128, 4, 128]), op=ALU.mult))
        else:
            wq = attn_w_qkv[i]
            dma(POOL, wds, [], [r_Win], lambda: G.dma_start(out=Win[:, :, 0:1024], in_=wq[:, 0:1024].rearrange("(kt p) n -> p kt n", p=128)))
            for kv in range(2):
                for dup in range(2):
                    dma(POOL, wds, [], [r_Win], lambda kv=kv, dup=dup: G.dma_start(out=Win[:, :, 1024 + kv * 128 + dup * 64:1024 + kv * 128 + (dup + 1) * 64], in_=wq[:, 1024 + kv * 64:1024 + (kv + 1) * 64].rearrange("(kt p) n -> p kt n", p=128)))
            dma(POOL, wds, [], [r_Win], lambda: G.dma_start(out=Win[:, :, 1280:1408], in_=wq[:, 1152:1280].rearrange("(kt p) n -> p kt n", p=128)))
            dma(POOL, wds, [], [r_Wout], lambda: G.dma_start(out=Wout[:], in_=attn_w_o[i].rearrange("(kt p) n -> p kt n", p=128)))
            dma(SP, lds, [], [r_qb], lambda: nc.sync.dma_start(out=qb[:].unsqueeze(2), in_=attn_b_qkv[i, 0:1024].rearrange("(j p o) -> p j o", p=128, o=1), allow_slow_non_contiguous=True))
            for kv in range(2):
                for dup in range(2):
                    dma(SP, lds, [], [r_kb2], lambda kv=kv, dup=dup: nc.sync.dma_start(out=kb2[dup * 64:(dup + 1) * 64, kv:kv + 1], in_=attn_b_qkv[i, 1024 + kv * 64:1024 + (kv + 1) * 64].rearrange("(p o) -> p o", o=1), allow_slow_non_contiguous=True))
            bcast_load(vbB[:], r_vbB, attn_b_qkv[i, 1152:1280])
            bcast_load(boB[:], r_boB, attn_b_o[i])
            bcast_load(sinkB[:], r_sinkB, attn_sinks[i])
            regroup(lds, [r_lnG, r_lnB, r_Wr, r_qb, r_kb2, r_vbB, r_boB, r_sinkB])
            regroup(wds, [r_Win, r_Wout])
            op(DVE, [r_qb], [r_qb], lambda: V.tensor_scalar(out=qb[:], in0=qb[:], scalar1=0.125, scalar2=None, op0=ALU.mult))

    def phase_M(l):
        load_layer_consts(l)
        load_expert(l, 0)
        load_expert(l, 1)
        srcD = x_in if l == 0 else X2
        r_srcD = [Res(f"xin{c}") for c in range(NCH)] if l == 0 else r_X2

        def issue_load(c):
            t, r = xt[c % 2]
            dma(SP, xtds[c % 2], [r_srcD[c]], [r], lambda: nc.sync.dma_start(out=t[:], in_=srcD[c * 128:(c + 1) * 128, :]))

        mixer = mixer_even if l % 2 == 0 else mixer_odd
        issue_load(0)
        transposes_x(*xt[0])
        mixer(l, 0, *xt[0], 0)
        for c in range(NCH):
            if c + 1 < NCH:
                issue_load(c + 1)
            src, r_src = xt[c % 2]
            mixer(l, c, src, r_src, 1)
            if c + 1 < NCH:
                transposes_x(*xt[(c + 1) % 2])
            layer_norm(tt, r_tt, x1, r_x1, lnG, r_lnG, lnB, r_lnB)
            if c + 1 < NCH:
                mixer(l, c + 1, *xt[(c + 1) % 2], 0)
            router_and_scatter(l, c)

    def phase_E(l):
        sc_tok = (sc_ds.sem, sc_ds.total)

        def load_xs(e):
            (xst, r_xst) = xs[e % 2]
            dma(SP, xsds[e % 2], [], [r_xst], lambda: nc.sync.dma_start(out=xst[:], in_=XS[e * CAP:(e + 1) * CAP, :].rearrange("(r p) d -> p r d", p=128)), extra=[sc_tok])

        for e in range(NEXP):
            s = e % 2
            (wg, r_wg), (wu, r_wu), (wd, r_wd) = Wg[s], Wu[s], Wd[s]
            (xst, r_xst) = xs[s]
            if e == 0:
                load_xs(0)
            if e + 1 < NEXP:
                load_xs(e + 1)
            for rb in range(RB):
                bt, r_bt = banks[rb % 2]
                btb = bt[:].bitcast(BF16)
                for kt in range(8):
                    op(PE, [r_xst, r_ident_b], [r_bt], lambda rb=rb, kt=kt, btb=btb: T.transpose(out=btb[:, kt * 128:(kt + 1) * 128], in_=xst[:, rb, kt * 128:(kt + 1) * 128], identity=ident_b[:]))
                if rb % 2 == 0:
                    op(ACT, [r_bt], [r_xsT], lambda rb=rb, btb=btb: S.copy(out=xsT[:, :, rb * 128:(rb + 1) * 128], in_=btb.rearrange("p (k t) -> p k t", k=8)))
                else:
                    op(DVE, [r_bt], [r_xsT], lambda rb=rb, btb=btb: V.tensor_copy(out=xsT[:, :, rb * 128:(rb + 1) * 128], in_=btb.rearrange("p (k t) -> p k t", k=8)))
            for hc in range(4):
                bg, r_bg = banks[2 + (hc % 2) * 2]
                bu, r_bu = banks[3 + (hc % 2) * 2]
                for kt in range(8):
                    op(PE, [r_wg, r_xsT], [r_bg], lambda hc=hc, kt=kt, bg=bg: T.matmul(bg[:, 0:CAP], lhsT=wg[:, kt, hc * 128:(hc + 1) * 128], rhs=xsT[:, kt, :], start=(kt == 0), stop=(kt == 7)))
                for kt in range(8):
                    op(PE, [r_wu, r_xsT], [r_bu], lambda hc=hc, kt=kt, bu=bu: T.matmul(bu[:, 0:CAP], lhsT=wu[:, kt, hc * 128:(hc + 1) * 128], rhs=xsT[:, kt, :], start=(kt == 0), stop=(kt == 7)))
                sgt, r_sgt = sg[hc % 2]
                op(ACT, [r_bg], [r_sgt], lambda bg=bg, sgt=sgt: S.activation(out=sgt[:], in_=bg[:, 0:CAP], func=AF.Silu))
                op(DVE, [r_bu, r_sgt], [r_hT], lambda hc=hc, bu=bu, sgt=sgt: V.tensor_tensor(out=hT[:, hc, :], in0=bu[:, 0:CAP], in1=sgt[:], op=ALU.mult))
            if e + 2 < NEXP:
                load_expert(l, e + 2, part=0)
            yst, r_yst = ys[s]
            for rb in range(RB):
                for half in range(2):
                    bt, r_bt = banks[6 + half]
                    for hc in range(4):
                        op(PE, [r_hT, r_wd], [r_bt], lambda rb=rb, half=half, hc=hc, bt=bt: T.matmul(bt[:, :], lhsT=hT[:, hc, rb * 128:(rb + 1) * 128], rhs=wd[:, hc, half * 512:(half + 1) * 512], start=(hc == 0), stop=(hc == 3)))
                    if half == 0:
                        op(ACT, [r_bt], [r_yst], lambda rb=rb, bt=bt: S.copy(out=yst[:, rb, 0:512], in_=bt[:, :]))
                    else:
                        op(DVE, [r_bt], [r_yst], lambda rb=rb, bt=bt: V.tensor_copy(out=yst[:, rb, 512:1024], in_=bt[:, :]))
            dma(SP, ysds[s], [r_yst], [], lambda: nc.sync.dma_start(out=YS[e * CAP:(e + 1) * CAP, :].rearrange("(r p) d -> p r d", p=128), in_=yst[:]))
            if e + 2 < NEXP:
                load_expert(l, e + 2, part=1)

    def phase_C(l, last):
        ys_toks = [(d.sem, d.total) for d in ysds]

        def issue(c):
            s = c % 2
            (a, r_a), (b, r_b), (xx, r_xx) = y1[s], y2[s], xc[s]
            dma(POOL, gds[s], [r_DI], [r_a], lambda: G.indirect_dma_start(out=a[:], out_offset=None, in_=YS, in_offset=bass.IndirectOffsetOnAxis(ap=DI[:, c, 0:1], axis=0)), extra=ys_toks)
            dma(POOL, gds[s], [r_DI], [r_b], lambda: G.indirect_dma_start(out=b[:], out_offset=None, in_=YS, in_offset=bass.IndirectOffsetOnAxis(ap=DI[:, c, 1:2], axis=0)), extra=ys_toks)
            regroup(gds[s], [r_a, r_b])
            dma(SP, xcds[s], [r_X1[c]], [r_xx], lambda: nc.sync.dma_start(out=xx[:], in_=X1[c * 128:(c + 1) * 128, :]))

        c0 = HALO if last else 0
        issue(c0)
        for c in range(c0, NCH):
            if c + 1 < NCH:
                issue(c + 1)
            s = c % 2
            (a, r_a), (b, r_b), (xx, r_xx) = y1[s], y2[s], xc[s]
            op(ACT, [r_a, r_RI], [r_a], lambda: S.activation(out=a[:], in_=a[:], func=AF.Identity, scale=RI[:, c, 0:1]))
            op(DVE, [r_b, r_RI, r_a], [r_a], lambda: V.scalar_tensor_tensor(out=a[:], in0=b[:], scalar=RI[:, c, 1:2], in1=a[:], op0=ALU.mult, op1=ALU.add))
            (ttc, r_ttc), (x2t, r_x2t) = ttc_[s], x2t_[s]
            op(DVE, [r_xx, r_a], [r_ttc], lambda: V.scalar_tensor_tensor(out=ttc[:], in0=xx[:], scalar=ALPHA, in1=a[:], op0=ALU.mult, op1=ALU.add))
            layer_norm(ttc, r_ttc, x2t, r_x2t, lnG, r_lnG, lnB, r_lnB, par=s)
            if dbg and HALO <= c < HALO + 8:
                dma(SP, st_ds, [r_x2t], [], lambda: nc.sync.dma_start(out=dbg_out[l, (c - HALO) * 128:(c - HALO + 1) * 128, :], in_=x2t[:]))
            if last:
                tok = dma(SP, st_ds, [r_x2t], [], lambda: nc.sync.dma_start(out=out[(c - HALO) * 128:(c - HALO + 1) * 128, :], in_=x2t[:]))
                out_toks.append(tok)
            else:
                dma(SP, st_ds, [r_x2t], [r_X2[c]], lambda: nc.sync.dma_start(out=X2[c * 128:(c + 1) * 128, :], in_=x2t[:]))

    op(DVE, [], [r_x1T], lambda: V.memset(x1T[:], 0.0))
    dma(SP, st_ds, [r_x1T], [], lambda: nc.sync.dma_start(out=YS[XS_ROWS:XS_ROWS + 128, :], in_=x1T[:].rearrange("p k t -> p (k t)")))
    for l in range(nlayers):
        phase_M(l)
        barrier()
        phase_E(l)
        barrier()
        bcast_load(lnG[:], r_lnG, ln2_g[l])
        bcast_load(lnB[:], r_lnB, ln2_b[l])
        regroup(lds, [r_lnG, r_lnB])
        phase_C(l, l == nlayers - 1)
        barrier()

    SP.wait((st_ds.sem, st_ds.total))
    nc.sync.wait_ge(st_ds.sem, st_ds.total)
    es.close()
    return nc


def _t5_bucket(rel):
    n = np.maximum(rel, 0)
    max_exact = 16
    nf = np.maximum(n, 1).astype(np.float32)
    large = max_exact + (np.log(nf / max_exact) / np.log(128 / max_exact) * (32 - max_exact)).astype(np.int32)
    large = np.minimum(large, 31)
    return np.where(n < max_exact, n, large)


def _consts(rel_bias_table):
    a = np.arange(128)[:, None]
    c = np.arange(256)[None, :]
    rel = a + 128 - c
    idx = _t5_bucket(rel)
    bias = np.asarray(rel_bias_table, np.float32)[idx]
    bias = np.ascontiguousarray(np.transpose(bias, (0, 2, 1)))
    win = (rel >= 0) & (rel < 128)
    bias[:, :, :][np.broadcast_to(~win[:, None, :], bias.shape)] = NEG
    ident = np.eye(128, dtype=np.float32)
    ustrict = np.triu(np.ones((128, 128), np.float32), 1)
    causal = np.triu(np.ones((128, 128), np.float32), 0)
    ecoff = np.broadcast_to((np.arange(32, dtype=np.float32) * CAP)[None, :], (128, 32)).copy()
    return bias, ident, ustrict, causal, ecoff


_CACHE = {}


def kernel(x, rel_bias_table, mix_w_in, gmlp_ln_g, gmlp_ln_b, gmlp_w_spatial, gmlp_b_spatial,
           conv_w, mix_w_out, attn_w_qkv, attn_b_qkv, attn_sinks, attn_w_o, attn_b_o,
           ln1_g, ln1_b, ln2_g, ln2_b, router_group, router_expert,
           expert_w_gate, expert_w_up, expert_w_down, _nlayers=DEPTH, _prep_only=False):
    f = lambda a: np.ascontiguousarray(np.asarray(a, dtype=np.float32))
    x = f(x)
    bias, ident, ustrict, causal, ecoff = _consts(rel_bias_table)
    shared = {
        "mix_w_in": f(mix_w_in), "mix_w_out": f(mix_w_out), "gmlp_ln_g": f(gmlp_ln_g),
        "gmlp_ln_b": f(gmlp_ln_b), "gmlp_w_spatial": f(gmlp_w_spatial),
        "gmlp_b_spatial": f(gmlp_b_spatial), "conv_w": f(conv_w),
        "attn_w_qkv": f(attn_w_qkv), "attn_b_qkv": f(attn_b_qkv), "attn_sinks": f(attn_sinks),
        "attn_w_o": f(attn_w_o), "attn_b_o": f(attn_b_o),
        "ln1_g": f(ln1_g), "ln1_b": f(ln1_b), "ln2_g": f(ln2_g), "ln2_b": f(ln2_b),
        "router_group": f(router_group), "router_expert": f(router_expert),
        "expert_w_gate": f(expert_w_gate), "expert_w_up": f(expert_w_up),
        "expert_w_down": f(expert_w_down),
        "attn_bias": bias, "c_ident": ident, "c_ustrict": ustrict, "c_causal": causal,
        "c_ecoff": ecoff,
    }
    in_maps = []
    per_seq = SEQ // (OWN * 128)
    for core in range(NCORES):
        b, q = core // per_seq, core % per_seq
        start = q * OWN * 128
        xin = np.zeros((TOK, D), np.float32)
        if q == 0:
            xin[HALO * 128:] = x[b, 0:OWN * 128]
        else:
            xin[:] = x[b, start - HALO * 128:start + OWN * 128]
        flags = np.zeros((128, 4), np.float32)
        flags[:, 2] = XS_ROWS + np.arange(128)
        flags[:, 3] = 1.0 if q == 0 else 0.0
        flags[:, 0] = 0.0 if q == 0 else 1.0
        flags[:, 1] = NEG if q == 0 else 0.0
        m = dict(shared)
        m["x_in"] = xin
        m["c_flags"] = flags
        in_maps.append(m)
    if _prep_only:
        return in_maps
    if _nlayers not in _CACHE:
        _CACHE[_nlayers] = build(_nlayers)
    nc = _CACHE[_nlayers]
    res = run_bass_kernel_spmd(nc, in_maps, core_ids=list(range(NCORES)))
    outs = [np.asarray(r["out"]) for r in res.results]
    y = np.stack(outs, 0).reshape(2, SEQ, D).astype(np.float32)
    return y
```
